# Optimizing a Trainium2 kernel written in Bass

```python
import math
import jax, jax.numpy as jnp
from jax import lax
import numpy as np

D_MODEL = 1024
BATCH = 8
SEQ = 4096
DEPTH = 1

CTX_LEN = 256
GRID_W = 64

GDN_HEADS = 4
GDN_HEAD_DIM = 128
GDN_WIDTH = GDN_HEADS * GDN_HEAD_DIM
CONV_W = 5
CHUNK = 64
ROPE_BASE = 10000.0

NA_HEADS = 8
NA_HEAD_DIM = 64
NA_WIDTH = NA_HEADS * NA_HEAD_DIM
NA_ROWS = 8
NA_COLS = 16

MIX_WIDTH = GDN_WIDTH + NA_WIDTH
IN_COLS = 4 * GDN_WIDTH + 4 * GDN_HEADS + 3 * NA_WIDTH

N_EXPERTS = 16
CAPACITY_FACTOR = 2
D_EXPERT = 1024

EPS = 1e-6

kernel_name = 'hybrid_gdn_natten_ec_dit_block'


def rms_norm(x, w):
    xf = x.astype(jnp.float32)
    y = xf * lax.rsqrt(jnp.mean(xf * xf, axis=-1, keepdims=True) + EPS)
    return y.astype(x.dtype) * w


def l2_normalize(t):
    tf = t.astype(jnp.float32)
    return (tf * lax.rsqrt(jnp.sum(tf * tf, axis=-1, keepdims=True) + EPS)).astype(t.dtype)


def modulate(h, shift, scale):
    return h * (1.0 + scale) + shift


def depthwise_conv(x, w):
    C = x.shape[-1]
    return lax.conv_general_dilated(
        x, w[:, None, :].astype(x.dtype), window_strides=(1,),
        padding=((CONV_W // 2, CONV_W // 2),),
        dimension_numbers=('NWC', 'WIO', 'NWC'), feature_group_count=C)


def axial_rope(x):
    T, D = x.shape[1], x.shape[-1]
    half = D // 2
    pairs = half // 2
    t = jnp.arange(T)
    inv_freq = ROPE_BASE ** (-jnp.arange(pairs, dtype=jnp.float32) / pairs)

    def rot(xa, pos):
        ang = pos.astype(jnp.float32)[:, None] * inv_freq[None, :]
        cos = jnp.cos(ang)[None, :, None, :].astype(x.dtype)
        sin = jnp.sin(ang)[None, :, None, :].astype(x.dtype)
        x1, x2 = xa[..., :pairs], xa[..., pairs:]
        return jnp.concatenate([x1 * cos - x2 * sin, x2 * cos + x1 * sin], axis=-1)

    return jnp.concatenate([rot(x[..., :half], t // GRID_W), rot(x[..., half:], t % GRID_W)], axis=-1)


def gdn_inputs(p_qkv, p_a, p_b, conv_w, a_log, dt_bias, rotary):
    B, T, _ = p_qkv.shape
    qkv = jax.nn.silu(depthwise_conv(p_qkv, conv_w))
    q, k, v = jnp.split(qkv, 3, axis=-1)
    heads = lambda t: t.reshape(B, T, GDN_HEADS, GDN_HEAD_DIM)
    q, k, v = l2_normalize(heads(q)), l2_normalize(heads(k)), heads(v)
    if rotary:
        q, k = axial_rope(q), axial_rope(k)
    a = p_a.astype(jnp.float32).reshape(B, T, 2, GDN_HEADS)
    b = p_b.astype(jnp.float32).reshape(B, T, 2, GDN_HEADS)
    g = -jnp.exp(a_log.astype(jnp.float32)) * jax.nn.softplus(a + dt_bias.astype(jnp.float32))
    beta = jax.nn.sigmoid(b)
    return q, k, v, g, beta


def chunk_gated_delta(q, k, v, g, beta, s0):
    B, H, T, dk = k.shape
    dv = v.shape[-1]
    n = T // CHUNK
    q = q * (dk ** -0.5)
    q, k, v = [t.reshape(B, H, n, CHUNK, t.shape[-1]) for t in (q, k, v)]
    g = g.reshape(B, H, n, CHUNK)
    beta = beta.reshape(B, H, n, CHUNK)
    gcum = jnp.cumsum(g, axis=-1)
    incl = jnp.tril(jnp.ones((CHUNK, CHUNK), dtype=bool))
    strict = jnp.tril(jnp.ones((CHUNK, CHUNK), dtype=bool), -1)
    decay = jnp.exp(jnp.where(incl, gcum[..., :, None] - gcum[..., None, :], -jnp.inf))
    kb = k * beta[..., None]
    lower = jnp.where(strict, jnp.einsum('bhnid,bhnjd->bhnij', kb, k) * decay, 0.0)
    eye = jnp.eye(CHUNK, dtype=jnp.float32)
    tinv = lax.linalg.triangular_solve(eye + lower, jnp.broadcast_to(eye, lower.shape),
                                       left_side=True, lower=True, unit_diagonal=True)
    u = tinv @ (v * beta[..., None])
    w = tinv @ (kb * jnp.exp(gcum)[..., None])
    attn = jnp.where(incl, jnp.einsum('bhnid,bhnjd->bhnij', q, k) * decay, 0.0)
    q_dec = q * jnp.exp(gcum)[..., None]
    k_dec = k * jnp.exp(gcum[..., -1:] - gcum)[..., None]
    g_last = jnp.exp(gcum[..., -1])
    xs = tuple(jnp.moveaxis(t, 2, 0) for t in (q_dec, k_dec, u, w, attn, g_last))

    def step(S, inp):
        qd, kd, ui, wi, ai, gl = inp
        v_new = ui - wi @ S
        o = qd @ S + ai @ v_new
        S = S * gl[..., None, None] + jnp.einsum('bhcd,bhce->bhde', kd, v_new)
        return S, o

    S, o = lax.scan(step, s0, xs)
    return jnp.moveaxis(o, 0, 2).reshape(B, H, T, dv), S


def bidirectional_gated_delta(lat, cin):
    q, k, v, g, beta = lat
    qc, kc, vc, gc, bc = cin
    B = q.shape[0]
    bhtd = lambda t: jnp.swapaxes(t, 1, 2).astype(jnp.float32)
    bht = lambda t, d: jnp.swapaxes(t[:, :, d, :], 1, 2).astype(jnp.float32)
    s0 = jnp.zeros((B, GDN_HEADS, GDN_HEAD_DIM, GDN_HEAD_DIM), jnp.float32)
    outs_l, outs_c = [], []
    for d in range(2):
        order = (lambda t: jnp.flip(t, axis=2)) if d == 1 else (lambda t: t)
        oc, s_ctx = chunk_gated_delta(order(bhtd(qc)), order(bhtd(kc)), order(bhtd(vc)),
                                      order(bht(gc, d)), order(bht(bc, d)), s0)
        ol, _ = chunk_gated_delta(order(bhtd(q)), order(bhtd(k)), order(bhtd(v)),
                                  order(bht(g, d)), order(bht(beta, d)), s_ctx)
        outs_l.append(order(ol))
        outs_c.append(order(oc))
    o_l = jnp.swapaxes(outs_l[0] + outs_l[1], 1, 2).astype(q.dtype)
    o_c = jnp.swapaxes(outs_c[0] + outs_c[1], 1, 2).astype(q.dtype)
    return o_l, o_c


def gated_rms_norm(o, gate, w):
    B, T, H, dv = o.shape
    y = rms_norm(o, w) * jax.nn.silu(gate.reshape(B, T, H, dv))
    return y.reshape(B, T, H * dv)


def neighbourhood_attention(q, k, v, kc, vc, rpb):
    B, T, H, dh = q.shape
    rows = T // GRID_W
    win_rows = min(NA_ROWS, rows)
    grid = lambda t: t.reshape(B, rows, GRID_W, H, dh)
    qg, kg, vg = grid(q * (dh ** -0.5)), grid(k), grid(v)
    col = jnp.arange(GRID_W)
    col_start = jnp.clip(col - NA_COLS // 2, 0, GRID_W - NA_COLS)
    col_in = (col[None, :] >= col_start[:, None]) & (col[None, :] < col_start[:, None] + NA_COLS)
    col_idx = jnp.clip(col[None, :] - col[:, None], -(NA_COLS - 1), NA_COLS - 1) + (NA_COLS - 1)
    rpb_f = rpb.astype(jnp.float32)
    n_lat = win_rows * GRID_W

    def row_block(r):
        start = jnp.clip(r - win_rows // 2, 0, rows - win_rows)
        kr = lax.dynamic_slice_in_dim(kg, start, win_rows, axis=1)
        vr = lax.dynamic_slice_in_dim(vg, start, win_rows, axis=1)
        qr = lax.dynamic_index_in_dim(qg, r, axis=1, keepdims=False)
        row_idx = start + jnp.arange(win_rows) - r + (NA_ROWS - 1)
        bias = rpb_f[:, row_idx][:, :, col_idx].transpose(0, 2, 1, 3)
        s_lat = jnp.einsum('bqhd,brkhd->bhqrk', qr, kr).astype(jnp.float32) + bias[None]
        s_lat = jnp.where(col_in[:, None, :], s_lat, -jnp.inf)
        s_ctx = jnp.einsum('bqhd,bchd->bhqc', qr, kc).astype(jnp.float32)
        s = jnp.concatenate([s_lat.reshape(B, H, GRID_W, n_lat), s_ctx], axis=-1)
        p = jax.nn.softmax(s, axis=-1).astype(q.dtype)
        p_lat = p[..., :n_lat].reshape(B, H, GRID_W, win_rows, GRID_W)
        p_ctx = p[..., n_lat:]
        return (jnp.einsum('bhqrk,brkhd->bqhd', p_lat, vr)
                + jnp.einsum('bhqc,bchd->bqhd', p_ctx, vc))

    out = lax.map(row_block, jnp.arange(rows))
    return jnp.moveaxis(out, 0, 1).reshape(B, T, H * dh)


def context_attention(qc, kc, vc):
    B, Tc, H, dh = qc.shape
    s = jnp.einsum('bqhd,bkhd->bhqk', qc * (dh ** -0.5), kc).astype(jnp.float32)
    p = jax.nn.softmax(s, axis=-1).astype(qc.dtype)
    return jnp.einsum('bhqk,bkhd->bqhd', p, vc).reshape(B, Tc, H * dh)


def token_mixer(h, hc, w_in, conv_qkv, a_log, dt_bias, gdn_norm, na_rpb, w_out, need_ctx):
    B, T, _ = h.shape
    Tc = hc.shape[1]
    cuts = (3 * GDN_WIDTH, 4 * GDN_WIDTH, 4 * GDN_WIDTH + 2 * GDN_HEADS, 4 * GDN_WIDTH + 4 * GDN_HEADS)
    qkv, gate, a, b, na = jnp.split(h @ w_in, cuts, axis=-1)
    qkv_c, gate_c, a_c, b_c, na_c = jnp.split(hc @ w_in, cuts, axis=-1)
    lat = gdn_inputs(qkv, a, b, conv_qkv, a_log, dt_bias, True)
    cin = gdn_inputs(qkv_c, a_c, b_c, conv_qkv, a_log, dt_bias, False)
    o_gdn, o_gdn_c = bidirectional_gated_delta(lat, cin)
    y_gdn = gated_rms_norm(o_gdn, gate, gdn_norm)
    na_heads = lambda t, n: t.reshape(B, n, 3, NA_HEADS, NA_HEAD_DIM)
    nl = na_heads(na, T)
    ncx = na_heads(na_c, Tc)
    y_na = neighbourhood_attention(nl[:, :, 0], nl[:, :, 1], nl[:, :, 2], ncx[:, :, 1], ncx[:, :, 2], na_rpb)
    y = jnp.concatenate([y_gdn, y_na], axis=-1) @ w_out
    if not need_ctx:
        return y, None
    y_c_gdn = gated_rms_norm(o_gdn_c, gate_c, gdn_norm)
    y_c_na = context_attention(ncx[:, :, 0], ncx[:, :, 1], ncx[:, :, 2])
    y_c = jnp.concatenate([y_c_gdn, y_c_na], axis=-1) @ w_out
    return y, y_c


def expert_choice_ffn(h, w_router, w_gate, w_up, w_down):
    B, T, D = h.shape
    cap = CAPACITY_FACTOR * T // N_EXPERTS
    aff = jax.nn.softmax((h @ w_router).astype(jnp.float32), axis=-1)
    gval, idx = lax.top_k(jnp.swapaxes(aff, 1, 2), cap)
    xe = jax.vmap(lambda hb, ib: hb[ib])(h, idx)
    hid = jax.nn.silu(jnp.einsum('becd,edf->becf', xe, w_gate)) * jnp.einsum('becd,edf->becf', xe, w_up)
    ye = jnp.einsum('becf,efd->becd', hid, w_down) * gval[..., None].astype(h.dtype)
    return jax.vmap(lambda yb, ib: jnp.zeros((T, D), yb.dtype).at[ib.reshape(-1)].add(yb.reshape(-1, D)))(ye, idx)


def setup_inputs(seed: int = 0) -> dict:
    key = jax.random.key(seed)
    ks = jax.random.split(key, 20)
    f32 = jnp.float32
    nrm = lambda k, shape, s: jax.random.normal(k, shape, f32) * s
    D = D_MODEL
    dt = jnp.exp(jax.random.uniform(ks[11], (DEPTH, 2, GDN_HEADS), f32, math.log(1e-3), math.log(1e-1)))
    return {
        'x': nrm(ks[0], (BATCH, SEQ, D), 1.0),
        'c': nrm(ks[1], (BATCH, D), 1.0),
        'ctx': nrm(ks[2], (BATCH, CTX_LEN, D), 1.0),
        'c_ctx': nrm(ks[3], (D,), 1.0),
        'w_mod': nrm(ks[4], (DEPTH, D, 6 * D), 0.5 * D ** -0.5),
        'b_mod': nrm(ks[5], (DEPTH, 6 * D), 0.02),
        'norm_mix': 1.0 + nrm(ks[6], (DEPTH, D), 0.1),
        'norm_ffn': 1.0 + nrm(ks[7], (DEPTH, D), 0.1),
        'w_in': nrm(ks[8], (DEPTH, D, IN_COLS), D ** -0.5),
        'conv_qkv': nrm(ks[9], (DEPTH, CONV_W, 3 * GDN_WIDTH), CONV_W ** -0.5),
        'a_log': jnp.log(jax.random.uniform(ks[10], (DEPTH, 2, GDN_HEADS), f32, 1.0, 16.0)),
        'dt_bias': dt + jnp.log(-jnp.expm1(-dt)),
        'gdn_norm': 1.0 + nrm(ks[12], (DEPTH, GDN_HEAD_DIM), 0.1),
        'na_rpb': nrm(ks[13], (DEPTH, NA_HEADS, 2 * NA_ROWS - 1, 2 * NA_COLS - 1), 0.2),
        'w_out': nrm(ks[14], (DEPTH, MIX_WIDTH, D), MIX_WIDTH ** -0.5),
        'w_router': nrm(ks[15], (DEPTH, D, N_EXPERTS), D ** -0.5),
        'w_gate': nrm(ks[16], (DEPTH, N_EXPERTS, D, D_EXPERT), D ** -0.5),
        'w_up': nrm(ks[17], (DEPTH, N_EXPERTS, D, D_EXPERT), D ** -0.5),
        'w_down': nrm(ks[18], (DEPTH, N_EXPERTS, D_EXPERT, D), D_EXPERT ** -0.5),
        'final_norm': 1.0 + nrm(ks[19], (D,), 0.1),
    }


def reference(x, c, ctx, c_ctx, w_mod, b_mod, norm_mix, norm_ffn, w_in, conv_qkv, a_log, dt_bias,
              gdn_norm, na_rpb, w_out, w_router, w_gate, w_up, w_down, final_norm):
    for li in range(DEPTH):
        need_ctx = li + 1 < DEPTH
        mod = jax.nn.silu(c) @ w_mod[li] + b_mod[li]
        mod_c = jax.nn.silu(c_ctx) @ w_mod[li] + b_mod[li]
        sh1, sc1, gt1, sh2, sc2, gt2 = jnp.split(mod[:, None, :], 6, axis=-1)
        sh1c, sc1c, gt1c, sh2c, sc2c, gt2c = jnp.split(mod_c, 6, axis=-1)
        h = modulate(rms_norm(x, norm_mix[li]), sh1, sc1)
        hc = modulate(rms_norm(ctx, norm_mix[li]), sh1c, sc1c)
        y, y_c = token_mixer(h, hc, w_in[li], conv_qkv[li], a_log[li], dt_bias[li], gdn_norm[li],
                             na_rpb[li], w_out[li], need_ctx)
        x = x + gt1 * y
        h2 = modulate(rms_norm(x, norm_ffn[li]), sh2, sc2)
        x = x + gt2 * expert_choice_ffn(h2, w_router[li], w_gate[li], w_up[li], w_down[li])
        if need_ctx:
            ctx = ctx + gt1c * y_c
            hc2 = modulate(rms_norm(ctx, norm_ffn[li]), sh2c, sc2c)
            ctx = ctx + gt2c * expert_choice_ffn(hc2, w_router[li], w_gate[li], w_up[li], w_down[li])
    return rms_norm(x, final_norm)
```

```python
import numpy as np
from contextlib import ExitStack
import concourse.bass as bass
import concourse.mybir as mybir
from concourse.bass_utils import run_bass_kernel_spmd
import ml_dtypes

F32 = mybir.dt.float32
BF16 = mybir.dt.bfloat16
U32 = mybir.dt.uint32
AF = mybir.ActivationFunctionType
ALU = mybir.AluOpType
AX = mybir.AxisListType

D = 1024
T = 4096
TC = 256
TT = T + TC
NCORES = 8
INC = 3600
GW = 512
NE = 16
CAP = 512
EPS = 1e-6
NEG = -30000.0


class Prog:
    ENG = ("pe", "dve", "act", "pool", "sp")

    def __init__(self, nc, n_dma_sems=48):
        self.nc = nc
        self.es = ExitStack()
        self.eng = dict(pe=nc.tensor, dve=nc.vector, act=nc.scalar, pool=nc.gpsimd, sp=nc.sync)
        self.csem = {e: self.es.enter_context(nc.semaphore("cs_" + e)) for e in self.ENG}
        self.ccnt = {e: 0 for e in self.ENG}
        self.dsem = [self.es.enter_context(nc.semaphore("ds%d" % i)) for i in range(n_dma_sems)]
        self.dcnt = [0] * n_dma_sems
        self.dpool = {"sp": list(range(0, 20)), "act": list(range(20, 28)), "pool": list(range(28, n_dma_sems))}
        self.dnext = {"sp": 0, "act": 0, "pool": 0}
        self.seen = {e: {} for e in self.ENG}
        self.recs = {}
        self.ninst = 0
        self.uid = 0

    def sb(self, stack, shape, dt, name=None):
        self.uid += 1
        return stack.enter_context(self.nc.sbuf_tensor("%s_%d" % (name or "t", self.uid), list(shape), dt))

    def ps(self, stack, shape, dt, name=None):
        self.uid += 1
        return stack.enter_context(self.nc.psum_tensor("%s_%d" % (name or "p", self.uid), list(shape), dt))

    def dram(self, name, shape, dt, kind="Internal"):
        if name in getattr(self, "dump", ()):
            kind = "ExternalOutput"
        return self.nc.dram_tensor(name, list(shape), dt, kind=kind).ap()

    def _sem(self, i):
        return self.csem[i[1]] if i[0] == "c" else self.dsem[i[1]]

    def _box(self, ap):
        t = ap.tensor
        pairs = [(int(s), int(c)) for s, c in ap.ap]
        off = int(ap.offset)
        if type(t).__name__.startswith("DRam"):
            ext = sum((c - 1) * abs(s) for s, c in pairs)
            return t.name, (0, 1, off, off + ext + 1)
        shp = [int(v) for v in t.shape]
        if type(t).__name__.startswith("PSum"):
            esz = 2 if ap.dtype == BF16 else 4
            fsz = 1
            for v in shp[1:]:
                fsz *= v
            f0 = off % fsz
            ext = sum((c - 1) * abs(s) for s, c in pairs[1:])
            b0 = (f0 * esz) // 2048
            b1 = ((f0 + ext + 1) * esz - 1) // 2048 + 1
            return "PS!" + t.name, (0, 128, b0 * 2048, b1 * 2048)
        fsz = 1
        for v in shp[1:]:
            fsz *= v
        ps_, pc = pairs[0]
        p0 = off // fsz
        f0 = off % fsz
        if ps_ != fsz:
            if ps_ == 0 or pc == 1:
                pc = 1
            else:
                return t.name, (0, 128, 0, fsz)
        ext = sum((c - 1) * abs(s) for s, c in pairs[1:])
        return t.name, (p0, p0 + pc, f0, f0 + ext + 1)

    def _deps(self, ap, is_write):
        name, (p0, p1, lo, hi) = self._box(ap)
        if name.startswith("PS!"):
            is_write = True
        out = []
        for r in self.recs.get(name, ()):
            if r[0] >= p1 or p0 >= r[1] or r[2] >= hi or lo >= r[3]:
                continue
            if is_write or r[4]:
                out.append(r[5])
        return out

    def _record(self, ap, is_write, ev):
        name, (p0, p1, lo, hi) = self._box(ap)
        if name.startswith("PS!"):
            is_write = True
        lst = self.recs.setdefault(name, [])
        keep = []
        for r in lst:
            covered = r[0] >= p0 and r[1] <= p1 and r[2] >= lo and r[3] <= hi
            if covered and (is_write or ((not r[4]) and r[5][0] == ev[0] and ev[0][0] == "c")):
                continue
            keep.append(r)
        keep.append((p0, p1, lo, hi, is_write, ev))
        self.recs[name] = keep

    def _wait(self, e, evs):
        need = {}
        for (i, v) in evs:
            if e == "pe" and i == ("c", "pe"):
                continue
            if self.seen[e].get(i, 0) < v and need.get(i, 0) < v:
                need[i] = v
        for i, v in need.items():
            self.eng[e].wait_ge(self._sem(i), v)
            self.seen[e][i] = v
            self.ninst += 1

    def _pre(self, e, reads, writes):
        evs = []
        for ap in reads:
            evs += self._deps(ap, False)
        for ap in writes:
            evs += self._deps(ap, True)
        self._wait(e, evs)

    def _post(self, e, ins, reads, writes):
        self.ccnt[e] += 1
        ins.then_inc(self.csem[e], 1)
        ev = (("c", e), self.ccnt[e])
        for ap in reads:
            self._record(ap, False, ev)
        for ap in writes:
            self._record(ap, True, ev)
        self.ninst += 1

    def dma(self, q, out, in_, extra_reads=(), **kw):
        reads = [in_] + list(extra_reads)
        writes = [out]
        evs = []
        for ap in reads:
            evs += self._deps(ap, False)
        for ap in writes:
            evs += self._deps(ap, True)
        slot = self._slot(q)
        if self.dcnt[slot] > 0:
            evs.append((("d", slot), self.dcnt[slot]))
        self._wait(q, evs)
        ins = self.eng[q].dma_start(out=out, in_=in_, **kw)
        self._dma_post(ins, slot, reads, writes)

    def _slot(self, q):
        lst = self.dpool[q]
        slot = lst[self.dnext[q] % len(lst)]
        self.dnext[q] += 1
        return slot

    def _dma_post(self, ins, slot, reads, writes):
        self.dcnt[slot] += 16
        ins.then_inc(self.dsem[slot], 16)
        ev = (("d", slot), self.dcnt[slot])
        for ap in reads:
            self._record(ap, False, ev)
        for ap in writes:
            self._record(ap, True, ev)
        self.ninst += 1

    def gather(self, out, src, idx):
        reads = [src, idx]
        writes = [out]
        evs = []
        for ap in reads:
            evs += self._deps(ap, False)
        evs += self._deps(out, True)
        slot = self._slot("pool")
        if self.dcnt[slot] > 0:
            evs.append((("d", slot), self.dcnt[slot]))
        self._wait("pool", evs)
        ins = self.nc.gpsimd.indirect_dma_start(out=out, out_offset=None, in_=src,
                                                in_offset=bass.IndirectOffsetOnAxis(ap=idx, axis=0))
        self._dma_post(ins, slot, reads, writes)

    def scatter_add(self, dst, src, idx, after=None):
        reads = [src, idx]
        evs = []
        for ap in reads:
            evs += self._deps(ap, False)
        if after is None:
            evs += self._deps(dst, True)
        else:
            evs += list(after)
        slot = self._slot("pool")
        if self.dcnt[slot] > 0:
            evs.append((("d", slot), self.dcnt[slot]))
        self._wait("pool", evs)
        ins = self.nc.gpsimd.indirect_dma_start(out=dst, out_offset=bass.IndirectOffsetOnAxis(ap=idx, axis=0),
                                                in_=src, in_offset=None, compute_op=ALU.add)
        self._dma_post(ins, slot, reads, [dst] if after is None else [])
        return (("d", slot), self.dcnt[slot])

    def mm(self, out, lhsT, rhs, start=True, stop=True):
        self._pre("pe", [lhsT, rhs], [out])
        ins = self.nc.tensor.matmul(out, lhsT=lhsT, rhs=rhs, start=start, stop=stop)
        self._post("pe", ins, [lhsT, rhs], [out])

    def tr(self, out, in_, ident):
        self._pre("pe", [in_, ident], [out])
        ins = self.nc.tensor.transpose(out, in_, ident)
        self._post("pe", ins, [in_, ident], [out])

    def act(self, out, in_, func, bias=None, scale=None, accum_out=None):
        reads = [in_]
        writes = [out]
        kw = {}
        if bias is not None:
            kw["bias"] = bias
            if not isinstance(bias, (int, float)):
                reads.append(bias)
        if scale is not None:
            kw["scale"] = scale
            if not isinstance(scale, (int, float)):
                reads.append(scale)
        if accum_out is not None:
            kw["accum_out"] = accum_out
            writes.append(accum_out)
        self._pre("act", reads, writes)
        ins = self.nc.scalar.activation(out=out, in_=in_, func=func, **kw)
        self._post("act", ins, reads, writes)

    def tt(self, e, out, in0, in1, op):
        self._pre(e, [in0, in1], [out])
        ins = self.eng[e].tensor_tensor(out=out, in0=in0, in1=in1, op=op)
        self._post(e, ins, [in0, in1], [out])

    def ts(self, e, out, in0, s1, s2, op0, op1=None, accum_out=None):
        reads = [in0]
        writes = [out]
        for s in (s1, s2):
            if s is not None and not isinstance(s, (int, float)):
                reads.append(s)
        kw = {}
        if op1 is not None:
            kw["op1"] = op1
        if accum_out is not None:
            kw["accum_out"] = accum_out
            writes.append(accum_out)
        self._pre(e, reads, writes)
        ins = self.eng[e].tensor_scalar(out=out, in0=in0, scalar1=s1, scalar2=s2, op0=op0, **kw)
        self._post(e, ins, reads, writes)

    def stt(self, e, out, in0, scalar, in1, op0, op1):
        reads = [in0, in1]
        if not isinstance(scalar, (int, float)):
            reads.append(scalar)
        assert e == "dve"
        self._pre(e, reads, [out])
        ins = self.eng[e].scalar_tensor_tensor(out=out, in0=in0, scalar=scalar, in1=in1, op0=op0, op1=op1)
        self._post(e, ins, reads, [out])

    def copy(self, e, out, in_):
        self._pre(e, [in_], [out])
        if e == "act":
            ins = self.nc.scalar.copy(out=out, in_=in_)
        else:
            ins = self.eng[e].tensor_copy(out=out, in_=in_)
        self._post(e, ins, [in_], [out])

    def memset(self, e, ap, val):
        self._pre(e, [], [ap])
        ins = self.eng[e].memset(ap, val)
        self._post(e, ins, [], [ap])

    def reduce(self, e, out, in_, op, axis=AX.X):
        self._pre(e, [in_], [out])
        ins = self.eng[e].tensor_reduce(out=out, in_=in_, axis=axis, op=op)
        self._post(e, ins, [in_], [out])

    def recip(self, out, in_):
        self._pre("dve", [in_], [out])
        ins = self.nc.vector.reciprocal(out=out, in_=in_)
        self._post("dve", ins, [in_], [out])

    def rsqrt(self, out, in_, scale=1.0, bias=0.0):
        self.act(out, in_, AF.Sqrt, bias=bias, scale=scale)
        self.recip(out, out)

    def max8(self, out, in_):
        self._pre("dve", [in_], [out])
        ins = self.nc.vector.max(out=out, in_=in_)
        self._post("dve", ins, [in_], [out])

    def max_index(self, out, in_max, in_values):
        self._pre("dve", [in_max, in_values], [out])
        ins = self.nc.vector.max_index(out=out, in_max=in_max, in_values=in_values)
        self._post("dve", ins, [in_max, in_values], [out])

    def match_replace(self, out, in_to_replace, in_values, imm):
        self._pre("dve", [in_to_replace, in_values], [out])
        ins = self.nc.vector.match_replace(out=out, in_to_replace=in_to_replace, in_values=in_values, imm_value=imm)
        self._post("dve", ins, [in_to_replace, in_values], [out])

    def barrier(self):
        evs = [(("c", e), self.ccnt[e]) for e in self.ENG if self.ccnt[e] > 0]
        evs += [(("d", i), self.dcnt[i]) for i in range(len(self.dsem)) if self.dcnt[i] > 0]
        for e in self.ENG:
            self._wait(e, [ev for ev in evs if ev[0] != ("c", e)])
        self.recs = {}

    def finish(self):
        evs = [(("c", e), self.ccnt[e]) for e in self.ENG if self.ccnt[e] > 0 and e != "sp"]
        evs += [(("d", i), self.dcnt[i]) for i in range(len(self.dsem)) if self.dcnt[i] > 0]
        self._wait("sp", evs)
        self.es.close()


def skew(phases, n):
    for it in range(n + len(phases) - 1):
        for k, ph in enumerate(phases):
            i = it - k
            if 0 <= i < n:
                ph(i)


def bcast_rows(ap_row, nparts):
    pairs = [(int(s), int(c)) for s, c in ap_row.ap]
    last = pairs[-1]
    return bass.AP(tensor=ap_row.tensor, offset=int(ap_row.offset), ap=[[0, nparts], [last[0], last[1]]])


def build(nc, upto=99, dbg=None):
    P = Prog(nc)
    dbg = dbg or {}
    P.dump = set(dbg.get("dump", ()))
    I = {}
    inp = lambda n, s, dt=F32: I.setdefault(n, nc.dram_tensor(n, list(s), dt, kind="ExternalInput").ap())
    x = inp("x", [T, D])
    ctx = inp("ctx", [TC, D])
    cc = inp("cc", [128, 8, 2])
    w_mod = inp("w_mod", [D, 6 * D])
    b_mod = inp("b_mod", [1, 6 * D])
    norm_mix = inp("norm_mix", [1, D])
    norm_ffn = inp("norm_ffn", [1, D])
    w_in = inp("w_in", [D, INC])
    ident_f = inp("ident_f", [128, 128])
    out = nc.dram_tensor("out", [T, D], F32, kind="ExternalOutput").ap()

    S_mod = P.dram("S_mod", [2, 6 * D], F32)
    S_qkvT = P.dram("S_qkvT", [3 * GW, TT], BF16)
    S_gate = P.dram("S_gate", [T, GW], F32)
    S_ab = P.dram("S_ab", [TT, 16], F32)
    S_naqT = P.dram("S_naqT", [8, 64, T], BF16)
    S_nakT = P.dram("S_nakT", [8, 64, TT], BF16)
    S_nav = P.dram("S_nav", [TT, GW], BF16)
    S_ymix = P.dram("S_ymix", [T, D], BF16)

    glob = ExitStack()
    identf = P.sb(glob, [128, 128], F32, "identf")
    identb = P.sb(glob, [128, 128], BF16, "identb")
    P.dma("sp", identf[:], ident_f[:, :])
    P.copy("dve", identb[:], identf[:])

    wstk = ExitStack()
    wsb = P.sb(wstk, [128, 8, INC], BF16, "wsb")
    for kc in range(8):
        for hf in range(2):
            P.dma("pool", wsb[:, kc, hf * 1800:(hf + 1) * 1800], w_in[kc * 128:(kc + 1) * 128, hf * 1800:(hf + 1) * 1800])
    with ExitStack() as st:
        cct = P.sb(st, [128, 8, 2], F32, "cct")
        sct = P.sb(st, [128, 8, 2], F32, "sct")
        bm = P.sb(st, [2, 6 * D], F32, "bm")
        mrow = P.sb(st, [2, 6 * D], F32, "mrow")
        wm = [P.sb(st, [128, 3 * D], F32, "wm") for _ in range(3)]
        mps = P.ps(st, [128, 3 * D], F32, "mps")
        P.dma("sp", cct[:], cc[:, :, :])
        P.dma("sp", bm[0:1, :], b_mod[:, :])
        P.dma("sp", bm[1:2, :], b_mod[:, :])
        P.act(sct[:], cct[:], AF.Silu)
        it = 0
        for nh in range(2):
            for kc in range(8):
                w = wm[it % 3]
                it += 1
                P.dma("sp" if it % 2 else "act", w[:], w_mod[kc * 128:(kc + 1) * 128, nh * 3 * D:(nh + 1) * 3 * D])
                for nb in range(6):
                    P.mm(mps[0:2, nb * 512:(nb + 1) * 512], sct[:, kc, :], w[:, nb * 512:(nb + 1) * 512],
                         start=(kc == 0), stop=(kc == 7))
            P.tt("dve", mrow[:, nh * 3 * D:(nh + 1) * 3 * D], mps[0:2, :], bm[:, nh * 3 * D:(nh + 1) * 3 * D], ALU.add)
        P.dma("sp", S_mod[:, :], mrow[:])
    P.barrier()
    if "mod" in dbg:
        return P, I

    bc = ExitStack()

    def bc_tile(row_ap, name):
        t = P.sb(bc, [128, D], F32, name)
        P.dma("sp", t[:], bcast_rows(row_ap, 128))
        return t

    sh1 = bc_tile(S_mod[0:1, 0:D], "sh1")
    gm1 = bc_tile(S_mod[0:1, D:2 * D], "gm1")
    sh1c = bc_tile(S_mod[1:2, 0:D], "sh1c")
    gm1c = bc_tile(S_mod[1:2, D:2 * D], "gm1c")
    nmx = bc_tile(norm_mix[0:1, :], "nmx")
    for g_ in (gm1, gm1c):
        P.stt("dve", g_[:], g_[:], 1.0, nmx[:], ALU.add, ALU.mult)

    with ExitStack() as st:
        xts = [P.sb(st, [128, D], F32, "xt") for _ in range(3)]
        junk = P.sb(st, [128, D], BF16, "junk")
        tmps = [P.sb(st, [128, D], F32, "tmp") for _ in range(2)]
        hbs = [P.sb(st, [128, D], BF16, "hb") for _ in range(2)]
        hTs = [P.sb(st, [128, 8, 512], BF16, "hT") for _ in range(2)]
        sss = [P.sb(st, [128, 2], F32, "ss") for _ in range(4)]
        stf = [P.sb(st, [128, 512], F32, "stf") for _ in range(4)]
        stb = [P.sb(st, [128, 512], BF16, "stb") for _ in range(4)]
        trp = [P.ps(st, [128, 8, 128], BF16, "trp") for _ in range(2)]
        pps = [P.ps(st, [128, 512], F32, "pps") for _ in range(4)]
        cnt = dict(x=0, t=0, p=0, sf=0, sb=0, ev=0)

        def evac(dst, src):
            cnt["ev"] += 1
            if cnt["ev"] % 2:
                P.act(dst, src, AF.Copy)
            else:
                P.copy("dve", dst, src)

        blocks = [(src, b0, 512) for src in ("x",) for b0 in range(0, T, 512)] + [("c", 0, 256)]
        hb8 = hbs + [P.sb(st, [128, D], BF16, "hb") for _ in range(6)]

        def prep_elem(bi):
            (srcn, b0, nt) = blocks[bi]
            src = x if srcn == "x" else ctx
            gmt, sht = (gm1, sh1) if srcn == "x" else (gm1c, sh1c)
            for i in range(nt // 128):
                xt = xts[cnt["x"] % 3]
                ss = sss[cnt["x"] % 4]
                tmp = tmps[cnt["x"] % 2]
                hb = hb8[(bi % 2) * 4 + i]
                cnt["x"] += 1
                P.dma("sp", xt[:], src[b0 + i * 128:b0 + (i + 1) * 128, :])
                P.act(junk[:], xt[:], AF.Square, accum_out=ss[:, 0:1])
                P.rsqrt(ss[:, 1:2], ss[:, 0:1], scale=1.0 / D, bias=EPS)
                P.stt("dve", tmp[:], xt[:], ss[:, 1:2], gmt[:], ALU.mult, ALU.mult)
                P.tt("pool", hb[:], tmp[:], sht[:], ALU.add)

        def prep_tr(bi):
            (srcn, b0, nt) = blocks[bi]
            hT = hTs[bi % 2]
            for i in range(nt // 128):
                hb = hb8[(bi % 2) * 4 + i]
                tp = trp[i % 2]
                for kc in range(8):
                    P.tr(tp[:, kc, :], hb[:, kc * 128:(kc + 1) * 128], identb[:])
                evac(hT[:, :, i * 128:(i + 1) * 128], tp[:, :, :])

        def mms(bi):
            (srcn, b0, nt) = blocks[bi]
            tok0 = b0 if srcn == "x" else T + b0
            hT = hTs[bi % 2]
            fm = [("qkv", c_) for c_ in range(12)] + ([("naq", c_) for c_ in range(4)] if srcn == "x" else []) + \
                 [("nak", c_) for c_ in range(4)]
            for (kind, c_) in fm:
                col0 = {"qkv": 0, "naq": 2064, "nak": 2064 + 512}[kind] + c_ * 128
                pp = pps[cnt["p"] % 4]
                cnt["p"] += 1
                for kc in range(8):
                    P.mm(pp[:, 0:nt], wsb[:, kc, col0:col0 + 128], hT[:, kc, 0:nt], start=(kc == 0), stop=(kc == 7))
                if kind == "qkv":
                    s_ = stb[cnt["sb"] % 4]
                    cnt["sb"] += 1
                    evac(s_[:, 0:nt], pp[:, 0:nt])
                    P.dma("sp", S_qkvT[c_ * 128:(c_ + 1) * 128, tok0:tok0 + nt], s_[:, 0:nt])
                else:
                    s_ = stb[cnt["sb"] % 4]
                    cnt["sb"] += 1
                    if kind == "naq":
                        P.act(s_[:, 0:nt], pp[:, 0:nt], AF.Copy, scale=0.125)
                    else:
                        evac(s_[:, 0:nt], pp[:, 0:nt])
                    dstT = S_naqT if kind == "naq" else S_nakT
                    for hh in range(2):
                        P.dma("sp", dstT[c_ * 2 + hh, :, tok0:tok0 + nt], s_[hh * 64:(hh + 1) * 64, 0:nt])
            for i in range(nt // 128):
                r0 = tok0 + i * 128
                groups = [("nav", 2064 + 1024, 512), ("ab", 2048, 16)] + ([("gate", 1536, 512)] if srcn == "x" else [])
                for (kind, col0, ncol) in groups:
                    pp = pps[cnt["p"] % 4]
                    cnt["p"] += 1
                    for kc in range(8):
                        P.mm(pp[:, 0:ncol], hT[:, kc, i * 128:(i + 1) * 128], wsb[:, kc, col0:col0 + ncol],
                             start=(kc == 0), stop=(kc == 7))
                    if kind == "nav":
                        s_ = stb[cnt["sb"] % 4]
                        cnt["sb"] += 1
                        evac(s_[:, 0:ncol], pp[:, 0:ncol])
                        P.dma("sp", S_nav[r0:r0 + 128, :], s_[:, 0:ncol])
                    else:
                        s_ = stf[cnt["sf"] % 4]
                        cnt["sf"] += 1
                        evac(s_[:, 0:ncol], pp[:, 0:ncol])
                        if kind == "ab":
                            P.dma("sp", S_ab[r0:r0 + 128, :], s_[:, 0:16])
                        else:
                            P.dma("sp", S_gate[r0:r0 + 128, :], s_[:, 0:512])

        prep_elem(0)
        prep_tr(0)
        for bi in range(len(blocks)):
            if bi + 1 < len(blocks):
                prep_elem(bi + 1)
            mms(bi)
            if bi + 1 < len(blocks):
                prep_tr(bi + 1)
    P.barrier()
    bc.close()
    wstk.close()
    if "proj" in dbg:
        return P, I
    build_gdn(P, nc, I, dbg, S_qkvT, S_gate, S_ab, S_ymix, identf, identb)
    if "gdn" in dbg:
        return P, I
    build_na(P, nc, I, dbg, S_naqT, S_nakT, S_nav, S_ymix)
    if "na" in dbg:
        return P, I
    build_tail(P, nc, I, dbg, x, out, S_mod, S_ymix, identf, identb)
    return P, I


def ap3(ap2d, mid=None, last=None):
    pairs = [(int(s_), int(c_)) for s_, c_ in ap2d.ap]
    assert len(pairs) == 2
    if mid is not None:
        ap = [list(pairs[0]), [0, mid], list(pairs[1])]
    else:
        ap = [list(pairs[0]), list(pairs[1]), [0, last]]
    return bass.AP(tensor=ap2d.tensor, offset=int(ap2d.offset), ap=ap)


NDT = F32


def build_gdn(P, nc, I, dbg, S_qkvT, S_gate, S_ab, S_ymix, identf, identb):
    inp = lambda n, s_, dt=F32: I.setdefault(n, nc.dram_tensor(n, list(s_), dt, kind="ExternalInput").ap())
    cmat_d = inp("cmat", [128, 8, 128])
    cwl_d = inp("cwl", [128, 12, 5])
    alog_d = inp("a_log", [1, 8])
    dtb_d = inp("dt_bias", [1, 8])
    gnorm_d = inp("gdn_norm", [1, 128])
    cos_d = inp("rope_cos", [128, T])
    sin_d = inp("rope_sin", [128, T])
    NP = TT // 128
    with ExitStack() as st:
        cm = P.sb(st, [128, 8, 128], F32, "cm")
        P.dma("sp", cm[:], cmat_d[:, :, :])
        Sm = [cm[:, 0, :], cm[:, 2, :]]
        Im = [cm[:, 1, :], cm[:, 3, :]]
        ones = cm[:, 4, :]
        perm = cm[:, 5, :]
        indA = cm[:, 6, :]
        indB = cm[:, 7, :]
        negm = P.sb(st, [128, 4, 128], BF16, "negm")
        for k_ in range(4):
            P.ts("dve", negm[:, k_, :], cm[:, k_, :], -1.0, -NEG, ALU.add, ALU.mult)
        negS = [negm[:, 0, :], negm[:, 2, :]]
        negI = [negm[:, 1, :], negm[:, 3, :]]
        posm = P.sb(st, [128, 2, 128], BF16, "posm")
        for k_, src_ in enumerate((0, 2)):
            P.ts("dve", posm[:, k_, :], cm[:, src_, :], -1.0, NEG, ALU.add, ALU.mult)
        posS = [posm[:, 0, :], posm[:, 1, :]]
        ones_b = P.sb(st, [128, 128], BF16, "ones_b")
        P.memset("pool", ones_b[:], 1.0)
        cwl = P.sb(st, [128, 12, 5], F32, "cwl")
        P.dma("sp", cwl[:], cwl_d[:, :, :])
        gnb = P.sb(st, [128, 128], F32, "gnb")
        P.dma("sp", gnb[:], bcast_rows(gnorm_d[0:1, :], 128))
        negA = P.sb(st, [128, 8], F32, "negA")
        dtb = P.sb(st, [128, 8], F32, "dtb")
        P.dma("sp", negA[:], bcast_rows(alog_d[0:1, :], 128))
        P.dma("sp", dtb[:], bcast_rows(dtb_d[0:1, :], 128))
        P.act(negA[:], negA[:], AF.Exp)
        P.ts("dve", negA[:], negA[:], -1.0, None, ALU.mult)
        banks = [P.ps(st, [128, 4, 128], F32, "gb") for _ in range(7)]
        trb = P.ps(st, [128, 8, 128], BF16, "gtr")
        cnt = dict(j=0, s=0, rr=0)

        def newbank():
            cnt["j"] += 1
            return banks[cnt["j"] % 4]

        ab_t = P.sb(st, [128, NP, 16], F32, "ab_t")
        P.dma("sp", ab_t[:], S_ab.rearrange("(n p) c -> p n c", p=128))
        g_t = P.sb(st, [128, NP, 8], F32, "g_t")
        nbeta_t = P.sb(st, [128, NP, 8], F32, "nbeta_t")
        beta_t = P.sb(st, [128, NP, 8], F32, "beta_t")
        bG_t = P.sb(st, [128, NP, 8], F32, "bG_t")
        Erem_t = P.sb(st, [128, NP, 8], F32, "Erem_t")
        Egl_t = P.sb(st, [128, 2, NP, 8], F32, "Egl_t")
        gc_t = P.sb(st, [128, NP, 8], F32, "gc_t")
        ngc_t = P.sb(st, [128, NP, 8], F32, "ngc_t")
        ghb_t = P.sb(st, [128, NP, 8], BF16, "ghb_t")
        ghf_t = P.sb(st, [128, NP, 8], F32, "ghf_t")
        glf_t = P.sb(st, [128, NP, 8], F32, "glf_t")
        P.tt("dve", g_t[:], ab_t[:, :, 0:8], ap3(dtb[:, :], mid=NP), ALU.add)
        P.act(g_t[:], g_t[:], AF.Exp)
        P.act(g_t[:], g_t[:], AF.Ln, bias=1.0, scale=1.0)
        P.tt("dve", g_t[:], g_t[:], ap3(negA[:, :], mid=NP), ALU.mult)
        P.act(beta_t[:], ab_t[:, :, 8:16], AF.Sigmoid)
        P.ts("dve", nbeta_t[:], beta_t[:], -1.0, None, ALU.mult)
        for d in range(2):
            for kind, mat, dst in (("cum", Im[d], bG_t), ("rem", Sm[d], Erem_t)):
                pq = banks[0]
                P.mm(pq[:, :, :].rearrange("p a b -> p (a b)")[:, 0:NP * 4].rearrange("p (n c) -> p n c", c=4),
                     mat, g_t[:, :, d * 4:(d + 1) * 4])
                pv_ = pq[:, :, :].rearrange("p a b -> p (a b)")[:, 0:NP * 4].rearrange("p (n c) -> p n c", c=4)
                if kind == "cum":
                    P.copy("dve", gc_t[:, :, d * 4:(d + 1) * 4], pv_)
                P.act(dst[:, :, d * 4:(d + 1) * 4], pv_, AF.Exp)
        for c_, ind in enumerate((indA, indB)):
            pq = banks[1]
            v = pq[:, :, :].rearrange("p a b -> p (a b)")[:, 0:NP * 8].rearrange("p (n c) -> p n c", c=8)
            P.mm(v, ind, g_t[:, :, :])
            P.act(Egl_t[:, c_, :, :], v, AF.Exp)
        P.ts("dve", ngc_t[:], gc_t[:], -1.0, None, ALU.mult)
        P.copy("dve", ghb_t[:], g_t[:])
        P.copy("dve", ghf_t[:], ghb_t[:])
        P.tt("dve", glf_t[:], g_t[:], ghf_t[:], ALU.subtract)
        P.copy("dve", ghb_t[:], glf_t[:])
        P.copy("dve", glf_t[:], ghb_t[:])
        P.tt("dve", bG_t[:], bG_t[:], beta_t[:], ALU.mult)
        if "gdnA" in dbg:
            for nm, t_ in (("D_g", g_t), ("D_beta", beta_t), ("D_bG", bG_t), ("D_Erem", Erem_t)):
                o_ = P.dram(nm, [128, NP, 8], F32)
                P.dma("sp", o_[:, :, :], t_[:])
            o_ = P.dram("D_Egl", [128, 2, NP, 8], F32)
            P.dma("sp", o_[:, :, :, :], Egl_t[:])
            return

        p_bs = [P.sb(st, [128, TT], BF16, "p_b") for _ in range(2)]
        diagws = [P.sb(st, [128, 5, 128], BF16, "diagw") for _ in range(2)]
        svq = [P.sb(st, [128, 512], F32, "svq") for _ in range(4)]
        svb = P.sb(st, [128, TT], BF16, "svb")
        qT = P.sb(st, [128, TT], BF16, "qT")
        kT = P.sb(st, [128, TT], BF16, "kT")
        k_tok = P.sb(st, [128, NP, 128], BF16, "k_tok")
        v_tok = P.sb(st, [128, NP, 128], BF16, "v_tok")
        o_acc = P.sb(st, [128, 32, 128], F32, "o_acc")
        gate_s = [P.sb(st, [128, 8, 128], F32, "gate_s") for _ in range(2)]
        ssq_s = [P.sb(st, [128, 8, 128], F32, "ssq_s") for _ in range(2)]
        blk = {n_: [P.sb(st, [128, 512], F32, n_) for _ in range(2)] for n_ in ("sqb", "rs", "qn", "t1", "t2", "cosb", "sinb")}
        RING = 4
        ring = {}
        for d in range(2):
            for r_ in range(RING):
                ring[(d, r_)] = dict(
                    wT=P.sb(st, [128, 128], BF16, "wT"), u=P.sb(st, [128, 128], F32, "u"),
                    attnT=P.sb(st, [128, 128], BF16, "attnT"), qdT=P.sb(st, [128, 128], BF16, "qdT"),
                    kdec=P.sb(st, [128, 128], BF16, "kdec"))
        NJ = 4
        jrg = [{n_: P.sb(st, [128, 128], BF16, n_) for n_ in ("rhsGh", "rhsGl")} for _ in range(4)]
        jt = [{n_: P.sb(st, [128, 128], F32, n_) for n_ in ("E", "ET", "Gbc")}
              for _ in range(NJ)]
        jtb = [{n_: P.sb(st, [128, 128], BF16, n_) for n_ in ("TTb", "vb", "kbd", "Qb", "Q2b")}
               for _ in range(NJ)]
        jU = [[P.sb(st, [128, 4, 128], BF16, "jU") for _ in range(2)] for _ in range(NJ)]
        for js_ in range(NJ):
            for u_ in jU[js_]:
                P.memset("pool", u_[:, 1, :], 0.0)
        Sst = [P.sb(st, [128, 128], F32, "Sst") for _ in range(2)]
        Sbf = [P.sb(st, [128, 128], BF16, "Sbf") for _ in range(2)]
        vn = [[P.sb(st, [128, 128], BF16, "vn") for _ in range(2)] for _ in range(2)]
        for d in range(2):
            for c_ in range(2):
                P.memset("pool", vn[d][c_][:], 0.0)
        rsn = P.sb(st, [128, 32], F32, "rsn")
        ybfs = [P.sb(st, [128, 8, 128], BF16, "ybf") for _ in range(2)]
        pending_epi = []

        def tokoff(n):
            return n * 128 if n < 32 else T + (n - 32) * 128

        def job_levels(h, d, n, slot, js):
            dh = d * 4 + h
            t0 = tokoff(n)
            J = jt[js]
            Jb = jtb[js]
            R = ring[(d, slot)]
            lat = n < 32
            gcol = g_t[:, n, dh:dh + 1]
            lv = []
            bk = {}
            U = jU[js]

            def a0():
                P.act(jrg[js]["rhsGh"][:], Im[d], AF.Copy, scale=ghf_t[:, n, dh:dh + 1])
                P.act(jrg[js]["rhsGl"][:], Im[d], AF.Copy, scale=glf_t[:, n, dh:dh + 1])
                P.tt("pool", Jb["vb"][:], v_tok[:, n, :], beta_t[:, n, dh:dh + 1].to_broadcast([128, 128]), ALU.mult)

            def a1():
                bA = newbank()
                bk["A"] = bA
                nq = 3 if lat else 1
                P.mm(bA[:, 0:nq, :], ones_b[:], ap3(jrg[js]["rhsGh"][:, :], mid=nq), start=True, stop=False)
                P.mm(bA[:, 0:nq, :], ones_b[:], ap3(jrg[js]["rhsGl"][:, :], mid=nq), start=False, stop=False)
                P.mm(bA[:, 0, :], identb[:], posS[d], start=False, stop=not lat)
                if lat:
                    P.mm(bA[:, 1, :], identb[:], negI[d], start=False, stop=True)
                P.mm(bA[:, 3, :], kT[:, t0:t0 + 128], kT[:, t0:t0 + 128])

            def a2():
                bA = bk["A"]
                P.act(J["E"][:], bA[:, 0, :], AF.Exp, bias=gc_t[:, n, dh:dh + 1], scale=-1.0)
                if lat:
                    P.act(J["ET"][:], bA[:, 1, :], AF.Exp, bias=ngc_t[:, n, dh:dh + 1], scale=1.0)
                    P.act(J["Gbc"][:], bA[:, 2, :], AF.Exp)
                P.tt("pool", Jb["kbd"][:], k_tok[:, n, :], bG_t[:, n, dh:dh + 1].to_broadcast([128, 128]), ALU.mult)

            def a3():
                P.stt("dve", Jb["Qb"][:], bk["A"][:, 3, :], nbeta_t[:, n, dh:dh + 1], J["E"][:], ALU.mult, ALU.mult)
                P.tt("pool", R["kdec"][:], k_tok[:, n, :], Erem_t[:, n, dh:dh + 1].to_broadcast([128, 128]), ALU.mult)

            def a4():
                bB = newbank()
                bk["B"] = bB
                P.tr(trb[:, js, :], Jb["Qb"][:], identb[:])
                if lat:
                    P.mm(bB[:, 0, :], kT[:, t0:t0 + 128], qT[:, t0:t0 + 128])

            def a5():
                P.copy("act", U[0][:, 2, :], trb[:, js, :])
                if lat:
                    P.tt("dve", R["attnT"][:], bk["B"][:, 0, :], J["ET"][:], ALU.mult)
                    P.tt("pool", R["qdT"][:], qT[:, t0:t0 + 128], J["Gbc"][:], ALU.mult)
            lv += [a0, a1, a2, a3, a4, a5]
            Qs = [Jb["Qb"], Jb["Q2b"]]
            for n_ in range(0, 6):
                Uc, Un = U[n_ % 2], U[(n_ + 1) % 2]
                Qc, Qn = Qs[n_ % 2], Qs[(n_ + 1) % 2]

                def m1(n_=n_, Uc=Uc, Qc=Qc):
                    bC = newbank()
                    bk[n_] = bC
                    if n_ < 5:
                        P.mm(bC[:, 0, :], Uc[:, 2, :], Qc[:])
                    if n_ == 0:
                        P.mm(bC[:, 2, :], Qc[:], Uc[:, 2, :])
                    elif n_ < 5:
                        P.mm(bC[:, 1:3, :], Qc[:], Uc[:, 0:3:2, :])
                    else:
                        P.mm(bC[:, 1, :], Qc[:], Uc[:, 0, :])

                def m2(n_=n_, Uc=Uc, Un=Un, Qn=Qn):
                    bC = bk[n_]
                    if n_ < 5:
                        P.copy("act", Qn[:], bC[:, 0, :])
                    if n_ == 0:
                        P.copy("act", Un[:, 2, :], bC[:, 2, :])
                        P.tt("pool", Un[:, 0, :], Uc[:, 2, :], identb[:], ALU.add)
                    elif n_ < 5:
                        P.tt("dve", Un[:, 0:3:2, :], bC[:, 1:3, :], Uc[:, 0:2, :], ALU.add)
                    else:
                        P.tt("dve", Jb["TTb"][:], bC[:, 1, :], Uc[:, 0, :], ALU.add)
                lv += [m1, m2]

            def n1():
                bD = newbank()
                bk["D"] = bD
                P.mm(bD[:, 0, :], Jb["TTb"][:], Jb["vb"][:])
                P.mm(bD[:, 1, :], Jb["kbd"][:], Jb["TTb"][:])

            def n2():
                P.copy("act", R["u"][:], bk["D"][:, 0, :])
                P.copy("act", R["wT"][:], bk["D"][:, 1, :])
            lv += [n1, n2]
            return lv

        def scan_levels(h, d, n, slot):
            dh = d * 4 + h
            R = ring[(d, slot)]
            lat = n < 32
            order = (0, 1) if d == 0 else (1, 0)
            lv = []
            for c_ in order:
                hs = slice(c_ * 64, (c_ + 1) * 64)
                p1, p3, p2 = banks[4 + d][:, 0, :], banks[4 + d][:, 1, :], banks[6][:, d, :]

                def A1(p1=p1):
                    P.mm(p1, R["wT"][:], Sbf[d][:])

                def A2(c_=c_, hs=hs, p1=p1):
                    P.tt("dve", vn[d][c_][hs, :], R["u"][hs, :], p1[hs, :], ALU.subtract)

                def B1(c_=c_, p2=p2, p3=p3):
                    P.mm(p3, R["kdec"][:], vn[d][c_][:])
                    if lat:
                        P.mm(p2, R["qdT"][:], Sbf[d][:], start=True, stop=False)
                        P.mm(p2, R["attnT"][:], vn[d][c_][:], start=False, stop=True)

                def B2(c_=c_, p3=p3):
                    P.stt("dve", Sst[d][:], Sst[d][:], Egl_t[:, c_, n, dh:dh + 1], p3, ALU.mult, ALU.add)

                def B3(hs=hs, p2=p2):
                    P.copy("act", Sbf[d][:], Sst[d][:])
                    if lat:
                        first = (n < 16) if d == 0 else (n >= 16)
                        if first:
                            P.copy("act", o_acc[hs, n, :], p2[hs, :])
                        else:
                            P.tt("dve", o_acc[hs, n, :], o_acc[hs, n, :], p2[hs, :], ALU.add)
                lv += [A1, A2, B1, B2, B3]
            return lv

        heads = dbg.get("gdn_heads", range(4))
        for h in heads:
            kinds = (2, 0, 1)
            blist = [(b0_, 512, 0, T) for b0_ in range(0, T, 512)] + [(T, TC, T, TC)]
            items = [(ki, bi) for ki in range(3) for bi in range(len(blist))]

            def load_kind(ki):
                c_ = kinds[ki] * 4 + h
                P.dma("sp", p_bs[ki % 2][:], S_qkvT[c_ * 128:(c_ + 1) * 128, :])
                for w in range(5):
                    P.act(diagws[ki % 2][:, w, :], identf[:], AF.Copy, scale=cwl[:, c_, w:w + 1])

            def bank512(k_):
                return banks[k_][:, :, :].rearrange("p a b -> p (a b)")

            def f0(i):
                ki, bi = items[i]
                if i == 0:
                    load_kind(0)
                if bi == 3 and ki + 1 < 3:
                    load_kind(ki + 1)
                b0, nb, seq0, L = blist[bi]
                p_b, dg = p_bs[ki % 2], diagws[ki % 2]
                pc = bank512(i % 2)
                P.mm(pc[:, 0:nb], dg[:, 2, :], p_b[:, b0:b0 + nb], start=True, stop=False)
                for w in (0, 1, 3, 4):
                    sh = w - 2
                    j_lo = max(0, seq0 - (b0 + sh))
                    j_hi = min(nb, seq0 + L - (b0 + sh))
                    P.mm(pc[:, j_lo:j_hi], dg[:, w, :], p_b[:, b0 + j_lo + sh:b0 + j_hi + sh], start=False, stop=(w == 4))

            def f1(i):
                ki, bi = items[i]
                b0, nb, seq0, L = blist[bi]
                pc = bank512(i % 2)
                if kinds[ki] == 2:
                    P.act(svb[:, b0:b0 + nb], pc[:, 0:nb], AF.Silu)
                    return
                P.act(svq[i % 4][:, 0:nb], pc[:, 0:nb], AF.Silu)
                P.act(blk["sqb"][i % 2][:, 0:nb], svq[i % 4][:, 0:nb], AF.Square)

            def f2a(i):
                ki, bi = items[i]
                b0, nb, seq0, L = blist[bi]
                if kinds[ki] == 2:
                    return
                pss = bank512(2 + i % 2)
                P.mm(pss[:, 0:nb], ones, blk["sqb"][i % 2][:, 0:nb])

            def f2b(i):
                ki, bi = items[i]
                b0, nb, seq0, L = blist[bi]
                if kinds[ki] == 2:
                    return
                pss = bank512(2 + i % 2)
                P.act(blk["rs"][i % 2][:, 0:nb], pss[:, 0:nb], AF.Sqrt, bias=EPS, scale=1.0)

            def f3a(i):
                ki, bi = items[i]
                b0, nb, seq0, L = blist[bi]
                if kinds[ki] == 2:
                    return
                dstT = qT if kinds[ki] == 0 else kT
                scale = 128 ** -0.5 if kinds[ki] == 0 else 1.0
                rs = blk["rs"][i % 2]
                P.recip(rs[:, 0:nb], rs[:, 0:nb])
                if b0 < T:
                    if not dbg.get("nocs"):
                        P.dma("sp", blk["cosb"][i % 2][:], cos_d[:, b0:b0 + 512])
                    P.stt("dve", blk["qn"][i % 2][:, 0:nb], svq[i % 4][:, 0:nb], scale, rs[:, 0:nb], ALU.mult, ALU.mult)
                else:
                    P.stt("dve", dstT[:, b0:b0 + nb], svq[i % 4][:, 0:nb], scale, rs[:, 0:nb], ALU.mult, ALU.mult)

            def f3b(i):
                ki, bi = items[i]
                b0, nb, seq0, L = blist[bi]
                if kinds[ki] == 2 or b0 >= T:
                    return
                qn = blk["qn"][i % 2]
                psr = bank512(4 + i % 2)
                P.mm(psr[:, 0:nb], perm, qn[:, 0:nb])
                P.tt("pool", blk["t1"][i % 2][:, 0:nb], qn[:, 0:nb], blk["cosb"][i % 2][:, 0:nb], ALU.mult)
                if not dbg.get("nocs"):
                    P.dma("sp", blk["sinb"][i % 2][:], sin_d[:, b0:b0 + 512])

            def f4(i):
                ki, bi = items[i]
                b0, nb, seq0, L = blist[bi]
                if kinds[ki] == 2 or b0 >= T:
                    return
                dstT = qT if kinds[ki] == 0 else kT
                psr = bank512(4 + i % 2)
                P.tt("dve", blk["t2"][i % 2][:, 0:nb], psr[:, 0:nb], blk["sinb"][i % 2][:, 0:nb], ALU.mult)
                P.tt("pool", dstT[:, b0:b0 + nb], blk["t1"][i % 2][:, 0:nb], blk["t2"][i % 2][:, 0:nb], ALU.add)

            phs_ = [f0, f1, f2a, f2b, f3a, f3b, f4]
            for it_ in range(len(items) + len(phs_) - 1):
                for k_, ph_ in enumerate(phs_):
                    i_ = it_ - k_
                    if 0 <= i_ < len(items):
                        ph_(i_)
                if pending_epi and it_ % 6 == 2:
                    pending_epi.pop(0)()
            while pending_epi:
                pending_epi.pop(0)()
            for (srcT, dst_tok) in ((svb, v_tok), (kT, k_tok)):
                for g0 in range(0, NP, 8):
                    ng = min(8, NP - g0)
                    for i_ in range(ng):
                        P.tr(trb[:, i_, :], srcT[:, (g0 + i_) * 128:(g0 + i_ + 1) * 128], identb[:])
                    P.copy("act", dst_tok[:, g0:g0 + ng, :], trb[:, 0:ng, :])
            if "gdnC" in dbg:
                for nm, t_, shp in (("D_qT", qT, [128, TT]), ("D_kT", kT, [128, TT]), ("D_vtok", v_tok, [128, NP, 128]),
                                    ("D_ktok", k_tok, [128, NP, 128])):
                    o_ = P.dram(nm, shp, BF16)
                    P.dma("sp", o_, t_[:])
                return
            for d in range(2):
                P.memset("pool", Sst[d][:], 0.0)
                P.memset("pool", Sbf[d][:], 0.0)
            seq = [[32, 33] + list(range(32)), [33, 32] + list(range(31, -1, -1))]
            prog = dict(jobs_done=set(), scan_done=-1)
            STAG = dbg.get("gdn_stagger", 2)

            def job_stream(par, delay):
                for _ in range(delay):
                    yield
                for s_ in range(par, NP, 2):
                    lvs = [job_levels(h, d, seq[d][s_], s_ % RING, par * 2 + d) for d in range(2)]
                    for li in range(len(lvs[0])):
                        for l_ in lvs:
                            if not dbg.get("gdn_nojobs"):
                                l_[li]()
                        yield
                    prog["jobs_done"].add(s_)

            def scan_stream():
                for s_ in range(NP):
                    while s_ not in prog["jobs_done"]:
                        yield
                    sl = [scan_levels(h, d, seq[d][s_], s_ % RING) for d in range(2)]
                    for li in range(len(sl[0])):
                        for l_ in sl:
                            if not dbg.get("gdn_noscan"):
                                l_[li]()
                        yield
                    prog["scan_done"] = s_

            SCN = dbg.get("gdn_scan_rate", 1)
            jstreams = [job_stream(0, 0), job_stream(1, STAG)]
            sstream = scan_stream()
            alive = [True, True]
            salive = [True]

            def step_scan():
                for _ in range(SCN):
                    if salive[0]:
                        try:
                            next(sstream)
                        except StopIteration:
                            salive[0] = False

            while any(alive) or salive[0]:
                for k_, g_ in enumerate(jstreams):
                    for rep in range(1):
                        if alive[k_]:
                            try:
                                next(g_)
                            except StopIteration:
                                alive[k_] = False
                        step_scan()
            if "gdnO" in dbg:
                o_ = P.dram("D_o%d" % h, [128, 32, 128], F32)
                P.dma("sp", o_, o_acc[:])
            def mk_epi(h, q4):
                def run():
                    n0 = q4 * 8
                    gt_, sq_ = gate_s[q4 % 2], ssq_s[q4 % 2]
                    oa = o_acc[:, n0:n0 + 8, :]
                    P.dma("sp", gt_[:], S_gate[n0 * 128:(n0 + 8) * 128, h * 128:(h + 1) * 128].rearrange("(n p) c -> p n c", p=128))
                    P.tt("dve", sq_[:], oa, oa, ALU.mult)
                    P.reduce("dve", rsn[:, n0:n0 + 8], sq_[:], ALU.add)
                    P.rsqrt(rsn[:, n0:n0 + 8], rsn[:, n0:n0 + 8], scale=1.0 / 128, bias=EPS)
                    P.tt("dve", sq_[:], oa, ap3(rsn[:, n0:n0 + 8], last=128), ALU.mult)
                    P.tt("pool", sq_[:], sq_[:], ap3(gnb[:, :], mid=8), ALU.mult)
                    P.act(gt_[:], gt_[:], AF.Silu)
                    P.tt("dve", ybfs[q4 % 2][:], sq_[:], gt_[:], ALU.mult)
                    P.dma("sp", S_ymix[n0 * 128:(n0 + 8) * 128, h * 128:(h + 1) * 128].rearrange("(n p) c -> p n c", p=128),
                          ybfs[q4 % 2][:])
                return run
            for q4 in range(4):
                pending_epi.append(mk_epi(h, q4))
            if h == list(heads)[-1]:
                while pending_epi:
                    pending_epi.pop(0)()
    P.barrier()


def na_tiles(a):
    if a == 0:
        return [(0 + i, i) for i in range(4)]
    if a == 1:
        return [(4 + i, i) for i in range(4)]
    if a == 30:
        return [(13 + i, 28 + i) for i in range(4)]
    if a == 31:
        return [(17 + i, 28 + i) for i in range(4)]
    return [(8 + i, a - 2 + i) for i in range(5)]


def build_na(P, nc, I, dbg, S_naqT, S_nakT, S_nav, S_ymix):
    inp = lambda n, s_, dt=F32: I.setdefault(n, nc.dram_tensor(n, list(s_), dt, kind="ExternalInput").ap())
    nab_d = inp("na_bias", [8, 128, 21, 128])
    nam_d = inp("na_mask", [128, 21, 128])
    NP = TT // 128
    with ExitStack() as st:
        mask = P.sb(st, [128, 21, 128], F32, "namask")
        P.dma("sp", mask[:], nam_d[:, :, :])
        BTs = [P.sb(st, [128, 21, 128], F32, "BT") for _ in range(2)]
        QTs = [P.sb(st, [64, T], BF16, "QT") for _ in range(2)]
        KTs = [P.sb(st, [64, TT], BF16, "KT") for _ in range(2)]
        V1s = [P.sb(st, [128, NP, 65], BF16, "V1") for _ in range(2)]
        for v_ in V1s:
            P.memset("pool", v_[:, :, 64:65], 1.0)
        yna = P.sb(st, [128, 32, 512], BF16, "yna")
        sbs = [P.sb(st, [128, 7, 128], F32, "nsb") for _ in range(2)]
        PTs = [P.sb(st, [128, 7, 128], BF16, "nPT") for _ in range(2)]
        rec = [P.sb(st, [128, 1], F32, "nrec") for _ in range(2)]
        stA = [P.ps(st, [128, 4, 128], F32, "stA") for _ in range(2)]
        stB = [P.ps(st, [128, 4, 128], F32, "stB") for _ in range(2)]
        pvo = [P.ps(st, [128, 512], F32, "pvo") for _ in range(2)]
        def head_loads(h):
            BT, QT, KT, V1 = BTs[h % 2], QTs[h % 2], KTs[h % 2], V1s[h % 2]
            P.dma("sp", BT[:], nab_d[h])
            P.dma("sp", QT[:], S_naqT[h])
            P.dma("sp", KT[:], S_nakT[h])
            P.dma("act", V1[:, :, 0:64], S_nav[:, h * 64:(h + 1) * 64].rearrange("(n p) c -> p n c", p=128))
            P.tt("pool", BT[:], BT[:], mask[:], ALU.add)

        def blkinfo(i):
            h, a = divmod(i, 32)
            tl = na_tiles(a)
            chunks = [m for (_, m) in tl] + [32, 33]
            return h, a, tl, len(tl), chunks

        def q0(i):
            h, a, tl, L, chunks = blkinfo(i)
            if i == 0:
                head_loads(0)
            if a == 8 and h + 1 < 8:
                head_loads(h + 1)
            QT, KT = QTs[h % 2], KTs[h % 2]
            sA, sB = stA[i % 2], stB[i % 2]
            q_ap = QT[:, a * 128:(a + 1) * 128]
            for j, m in enumerate(chunks):
                k0 = m * 128 if m < 32 else T + (m - 32) * 128
                dst = sA[:, j, :] if j < 4 else sB[:, j - 4, :]
                P.mm(dst, KT[:, k0:k0 + 128], q_ap)

        def q1(i):
            h, a, tl, L, chunks = blkinfo(i)
            BT = BTs[h % 2]
            sA, sB, sb_ = stA[i % 2], stB[i % 2], sbs[i % 2]
            t0 = tl[0][0]
            P.stt("dve", sb_[:, 0:4, :], sA[:, 0:4, :], 60.0, BT[:, t0:t0 + 4, :], ALU.min, ALU.add)
            if L == 5:
                P.stt("dve", sb_[:, 4:5, :], sB[:, 0:1, :], 60.0, BT[:, t0 + 4:t0 + 5, :], ALU.min, ALU.add)
                P.ts("dve", sb_[:, 5:7, :], sB[:, 1:3, :], 60.0, None, ALU.min)
            else:
                P.ts("dve", sb_[:, 4:6, :], sB[:, 0:2, :], 60.0, None, ALU.min)

        def q2(i):
            h, a, tl, L, chunks = blkinfo(i)
            P.act(PTs[i % 2][:, 0:L + 2, :], sbs[i % 2][:, 0:L + 2, :], AF.Exp)

        def q3(i):
            h, a, tl, L, chunks = blkinfo(i)
            V1, PT, po = V1s[h % 2], PTs[i % 2], pvo[i % 2]
            for j, m in enumerate(chunks):
                P.mm(po[:, 0:65], PT[:, j, :], V1[:, m, :], start=(j == 0), stop=(j == L + 1))

        def q4(i):
            h, a, tl, L, chunks = blkinfo(i)
            po = pvo[i % 2]
            P.recip(rec[i % 2][:], po[:, 64:65])
            P.act(yna[:, a, h * 64:(h + 1) * 64], po[:, 0:64], AF.Copy, scale=rec[i % 2][:, 0:1])

        skew([q0, q1, q2, q3, q4], 8 * 32)
        P.dma("sp", S_ymix[:, 512:1024].rearrange("(n p) c -> p n c", p=128), yna[:])
    P.barrier()


def build_tail(P, nc, I, dbg, x, out, S_mod, S_ymix, identf, identb):
    inp = lambda n, s_, dt=F32: I.setdefault(n, nc.dram_tensor(n, list(s_), dt, kind="ExternalInput").ap())
    w_out = inp("w_out", [D, D])
    wr_d = inp("w_router_l", [128, 8, NE])
    norm_ffn = I["norm_ffn"]
    fnorm_d = inp("final_norm", [1, D])
    w_gate = inp("w_gate", [NE, D, D])
    w_up = inp("w_up", [NE, D, D])
    w_down = inp("w_down", [NE, D, D])
    S_x1 = P.dram("S_x1", [T, D], F32)
    S_h2 = P.dram("S_h2", [T, D], BF16)
    keep = ExitStack()
    affT = P.sb(keep, [NE, T], F32, "affT")
    gt2 = P.sb(keep, [128, D], F32, "gt2")
    idxT = P.sb(keep, [128, 4, NE], U32, "idxT")
    valT = P.sb(keep, [128, 4, NE], F32, "valT")
    P.dma("sp", gt2[:], bcast_rows(S_mod[0:1, 5 * D:6 * D], 128))
    with ExitStack() as st:
        def bc_tile(row_ap, name):
            t_ = P.sb(st, [128, D], F32, name)
            P.dma("sp", t_[:], bcast_rows(row_ap, 128))
            return t_
        gt1 = bc_tile(S_mod[0:1, 2 * D:3 * D], "gt1")
        sh2 = bc_tile(S_mod[0:1, 3 * D:4 * D], "sh2")
        gm2 = bc_tile(S_mod[0:1, 4 * D:5 * D], "gm2")
        nf = bc_tile(norm_ffn[0:1, :], "nf")
        P.stt("dve", gm2[:], gm2[:], 1.0, nf[:], ALU.add, ALU.mult)
        wo = P.sb(st, [128, 8, D], BF16, "wo")
        for kc in range(8):
            P.dma("pool", wo[:, kc, :], w_out[kc * 128:(kc + 1) * 128, :])
        wr = P.sb(st, [128, 8, NE], F32, "wr")
        P.dma("sp", wr[:], wr_d[:, :, :])
        RG = 4
        yms = [P.sb(st, [128, D], BF16, "ym") for _ in range(RG)]
        ymT = [P.sb(st, [128, 8, 128], BF16, "ymT") for _ in range(RG)]
        xts = [P.sb(st, [128, D], F32, "xt4") for _ in range(RG)]
        x1s = [P.sb(st, [128, D], F32, "x1") for _ in range(RG)]
        tmps = [P.sb(st, [128, D], F32, "tmp4") for _ in range(RG)]
        h2s = [P.sb(st, [128, D], F32, "h2") for _ in range(RG)]
        h2b = [P.sb(st, [128, D], BF16, "h2b") for _ in range(RG)]
        h2T = [P.sb(st, [128, 8, 128], F32, "h2T") for _ in range(2)]
        junk = P.sb(st, [128, D], BF16, "junk4")
        sml = [P.sb(st, [128, 8], F32, "sml") for _ in range(RG)]
        lg = [P.sb(st, [128, NE], F32, "lg") for _ in range(RG)]
        trp = P.ps(st, [128, 8, 128], BF16, "trp4")
        yps = [P.ps(st, [128, 512], F32, "yps") for _ in range(2)]
        tps = [P.ps(st, [128, 4, 128], F32, "tps") for _ in range(2)]
        lps = [P.ps(st, [128, 512], F32, "lps") for _ in range(2)]
        aps_ = P.ps(st, [128, 512], F32, "aps")

        def pA0(i):
            r_ = i % RG
            P.dma("sp", yms[r_][:], S_ymix[i * 128:(i + 1) * 128, :])
            P.dma("sp", xts[r_][:], x[i * 128:(i + 1) * 128, :])

        def pA1(i):
            r_ = i % RG
            ym, yT = yms[r_], ymT[r_]
            for kc in range(8):
                P.tr(trp[:, kc, :], ym[:, kc * 128:(kc + 1) * 128], identb[:])
            P.copy("act", yT[:], trp[:])

        def pA2(i):
            r_ = i % RG
            yT, xt, x1, tmp = ymT[r_], xts[r_], x1s[r_], tmps[r_]
            for hf in range(2):
                for kc in range(8):
                    P.mm(yps[hf][:], yT[:, kc, :], wo[:, kc, hf * 512:(hf + 1) * 512], start=(kc == 0), stop=(kc == 7))
                P.tt("dve", tmp[:, hf * 512:(hf + 1) * 512], yps[hf][:], gt1[:, hf * 512:(hf + 1) * 512], ALU.mult)
            P.tt("pool", x1[:], tmp[:], xt[:], ALU.add)

        def pA3(i):
            r_ = i % RG
            x1, sm = x1s[r_], sml[r_]
            P.dma("sp", S_x1[i * 128:(i + 1) * 128, :], x1[:])
            P.act(junk[:], x1[:], AF.Square, accum_out=sm[:, 0:1])
            P.act(sm[:, 1:2], sm[:, 0:1], AF.Sqrt, bias=EPS, scale=1.0 / D)

        def pA4(i):
            r_ = i % RG
            x1, sm, tmp, h2, hb = x1s[r_], sml[r_], tmps[r_], h2s[r_], h2b[r_]
            P.recip(sm[:, 1:2], sm[:, 1:2])
            P.stt("dve", tmp[:], x1[:], sm[:, 1:2], gm2[:], ALU.mult, ALU.mult)
            P.tt("pool", h2[:], tmp[:], sh2[:], ALU.add)
            P.copy("pool", hb[:], h2[:])

        def pB1(i):
            r_ = i % RG
            h2, hT = h2s[r_], h2T[i % 2]
            P.dma("sp", S_h2[i * 128:(i + 1) * 128, :], h2b[r_][:])
            for kc in range(8):
                P.tr(tps[kc // 4][:, kc % 4, :], h2[:, kc * 128:(kc + 1) * 128], identf[:])
            P.copy("act", hT[:, 0:4, :], tps[0][:])
            P.copy("dve", hT[:, 4:8, :], tps[1][:])

        def pB2(i):
            r_ = i % RG
            hT, sm, lp = h2T[i % 2], sml[r_], lps[i % 2]
            for kc in range(8):
                P.mm(lp[:, 0:NE], hT[:, kc, :], wr[:, kc, :], start=(kc == 0), stop=(kc == 7))
            P.reduce("dve", sm[:, 2:3], lp[:, 0:NE], ALU.max)
            P.ts("dve", sm[:, 3:4], sm[:, 2:3], -1.0, None, ALU.mult)

        def pB3(i):
            r_ = i % RG
            sm, lp = sml[r_], lps[i % 2]
            P.act(lg[r_][:], lp[:, 0:NE], AF.Exp, bias=sm[:, 3:4], scale=1.0, accum_out=sm[:, 4:5])

        def pB4(i):
            r_ = i % RG
            sm = sml[r_]
            P.recip(sm[:, 5:6], sm[:, 4:5])
            P.ts("dve", lg[r_][:], lg[r_][:], sm[:, 5:6], None, ALU.mult)
            P.tr(aps_[0:NE, 0:128], lg[r_][:], identf[:])
            P.copy("act", affT[:, i * 128:(i + 1) * 128], aps_[0:NE, 0:128])

        skew([pA0, pA1, pA2, pA3, pA4, pB1, pB2, pB3, pB4], T // 128)
    P.barrier()
    if "tail4" in dbg:
        o_ = P.dram("D_affT", [NE, T], F32)
        P.dma("sp", o_, affT[:])
        return
    wst = ExitStack()
    wg = [P.sb(wst, [128, 8, D], BF16, "wg") for _ in range(2)]
    wu = [P.sb(wst, [128, 8, D], BF16, "wu") for _ in range(2)]
    wd = [P.sb(wst, [128, 8, D], BF16, "wd") for _ in range(2)]
    nexp = dbg.get("nexp", NE)

    def load_w(e):
        s_ = e % 2
        for (dst, src) in ((wg[s_], w_gate), (wu[s_], w_up), (wd[s_], w_down)):
            for hf in range(2):
                P.dma("pool", dst[:, hf * 4:(hf + 1) * 4, :],
                      src[e, hf * 512:(hf + 1) * 512, :].rearrange("(kc p) f -> p kc f", p=128))

    if "tail4" not in dbg and "topk" not in dbg:
        load_w(0)
        if nexp > 1:
            load_w(1)
    with ExitStack() as st:
        vals = P.sb(st, [NE, CAP], F32, "vals")
        idxs = P.sb(st, [NE, CAP], U32, "idxs")
        idxf = P.sb(st, [NE, CAP], F32, "idxf")
        S_aff = P.dram("S_aff", [NE, T], F32)
        S_cand = P.dram("S_cand", [128, 128], F32)
        P.dma("sp", S_aff[:, :], affT[:])
        a1 = P.sb(st, [128, 512], F32, "a1")
        c1 = P.sb(st, [128, 128], F32, "c1")
        c2 = P.sb(st, [NE, 1024], F32, "c2")
        P.dma("sp", a1[:], S_aff.rearrange("e (s t) -> (e s) t", t=512))
        for r_ in range(16):
            P.max8(c1[:, r_ * 8:(r_ + 1) * 8], a1[:])
            if r_ < 15:
                P.match_replace(a1[:], c1[:, r_ * 8:(r_ + 1) * 8], a1[:], -1.0)
        P.dma("sp", S_cand[:, :], c1[:])
        P.dma("sp", c2[:], S_cand.rearrange("(e s) r -> e (s r)", s=8))
        for r_ in range(CAP // 8):
            P.max8(vals[:, r_ * 8:(r_ + 1) * 8], c2[:])
            if r_ < CAP // 8 - 1:
                P.match_replace(c2[:], vals[:, r_ * 8:(r_ + 1) * 8], c2[:], -1.0)
        S_vals = P.dram("S_vals", [NE, CAP], F32)
        S_idx = P.dram("S_idx", [128, 64], U32)
        affrep = P.sb(st, [128, T], F32, "affrep")
        vals2 = P.sb(st, [128, 64], F32, "vals2")
        idx2 = P.sb(st, [128, 64], U32, "idx2")
        for r_ in range(8):
            P.dma("sp" if r_ % 2 else "act", affrep[r_::8, :], S_aff[:, :])
        P.dma("sp", S_vals[:, :], vals[:])
        P.dma("sp", vals2[:], S_vals.rearrange("e (r c) -> (e r) c", c=64))
        for g_ in range(8):
            P.max_index(idx2[:, g_ * 8:(g_ + 1) * 8], vals2[:, g_ * 8:(g_ + 1) * 8], affrep[:])
        P.dma("sp", S_idx[:, :], idx2[:])
        P.dma("sp", idxs[:], S_idx.rearrange("(e r) c -> e (r c)", r=8))
        P.copy("dve", idxf[:], idxs[:])
        tp = P.ps(st, [128, 512], F32, "tpk")
        for j in range(4):
            P.tr(tp[:, j * NE:(j + 1) * NE], idxf[:, j * 128:(j + 1) * 128], identf[0:NE, 0:NE])
        P.copy("dve", idxT[:], tp[:, 0:4 * NE].rearrange("p (j e) -> p j e", e=NE))
        for j in range(4):
            P.tr(tp[:, j * NE:(j + 1) * NE], vals[:, j * 128:(j + 1) * 128], identf[0:NE, 0:NE])
        P.copy("dve", valT[:], tp[:, 0:4 * NE].rearrange("p (j e) -> p j e", e=NE))
    P.barrier()
    if "topk" in dbg:
        o_ = P.dram("D_idxT", [128, 4, NE], U32)
        P.dma("sp", o_, idxT[:])
        o_ = P.dram("D_valT", [128, 4, NE], F32)
        P.dma("sp", o_, valT[:])
        return
    with ExitStack() as st:
        xg = [[P.sb(st, [128, D], BF16, "xg") for _ in range(4)] for _ in range(2)]
        xT = [P.sb(st, [128, 8, CAP], BF16, "xT") for _ in range(2)]
        hid = [P.sb(st, [128, 8, CAP], BF16, "hid") for _ in range(2)]
        sg = [P.sb(st, [128, CAP], F32, "sg") for _ in range(2)]
        ye = [P.sb(st, [128, D], F32, "ye") for _ in range(3)]
        trp = [P.ps(st, [128, 8, 128], BF16, "trpm") for _ in range(2)]
        pg = [P.ps(st, [128, 512], F32, "pg") for _ in range(2)]
        pu = [P.ps(st, [128, 512], F32, "pu") for _ in range(2)]
        py = [P.ps(st, [128, 512], F32, "py") for _ in range(2)]
        cnt = dict(y=0, p=0)
        sc_prev = []
        sc_cur = []

        def ph_g(e):
            for j in range(4):
                P.gather(xg[e % 2][j][:], S_h2[:, :], idxT[:, j, e:e + 1])

        def ph_t(e):
            for j in range(4):
                tpp = trp[j % 2]
                for kc in range(8):
                    P.tr(tpp[:, kc, :], xg[e % 2][j][:, kc * 128:(kc + 1) * 128], identb[:])
                if j % 2:
                    P.copy("act", xT[e % 2][:, :, j * 128:(j + 1) * 128], tpp[:])
                else:
                    P.copy("dve", xT[e % 2][:, :, j * 128:(j + 1) * 128], tpp[:])

        def ph_u(e):
            s_ = e % 2
            for fc in range(8):
                r_ = fc % 2
                for kc in range(8):
                    P.mm(pg[r_][:], wg[s_][:, kc, fc * 128:(fc + 1) * 128], xT[s_][:, kc, :], start=(kc == 0), stop=(kc == 7))
                for kc in range(8):
                    P.mm(pu[r_][:], wu[s_][:, kc, fc * 128:(fc + 1) * 128], xT[s_][:, kc, :], start=(kc == 0), stop=(kc == 7))
                P.act(sg[r_][:], pg[r_][:], AF.Silu)
                P.tt("dve", hid[s_][:, fc, :], sg[r_][:], pu[r_][:], ALU.mult)

        def ph_d(e):
            s_ = e % 2
            sc_prev[:] = sc_cur
            sc_cur[:] = []
            for j in range(4):
                y_ = ye[cnt["y"] % 3]
                cnt["y"] += 1
                for hf in range(2):
                    cnt["p"] += 1
                    pp = py[cnt["p"] % 2]
                    for fc in range(8):
                        P.mm(pp[:], hid[s_][:, fc, j * 128:(j + 1) * 128], wd[s_][:, fc, hf * 512:(hf + 1) * 512],
                             start=(fc == 0), stop=(fc == 7))
                    P.stt("dve", y_[:, hf * 512:(hf + 1) * 512], pp[:], valT[:, j, e:e + 1], gt2[:, hf * 512:(hf + 1) * 512],
                          ALU.mult, ALU.mult)
                sc_cur.append(P.scatter_add(S_x1[:, :], y_[:], idxT[:, j, e:e + 1], after=list(sc_prev)))

        ph_g(0)
        ph_t(0)
        for e in range(nexp):
            if e + 1 < nexp:
                ph_g(e + 1)
            ph_u(e)
            if e + 1 < nexp:
                ph_t(e + 1)
            ph_d(e)
            if e + 2 < nexp:
                load_w(e + 2)
    P.barrier()
    wst.close()
    if "moe" in dbg:
        return
    with ExitStack() as st:
        fn = P.sb(st, [128, D], F32, "fn")
        P.dma("sp", fn[:], bcast_rows(fnorm_d[0:1, :], 128))
        xs = [P.sb(st, [128, D], F32, "xf") for _ in range(4)]
        os_ = [P.sb(st, [128, D], F32, "of") for _ in range(3)]
        junk = P.sb(st, [128, D], BF16, "junkf")
        sm = [P.sb(st, [128, 2], F32, "smf") for _ in range(4)]

        def z0(i):
            P.dma("sp", xs[i % 4][:], S_x1[i * 128:(i + 1) * 128, :])

        def z1(i):
            s_ = sm[i % 4]
            P.act(junk[:], xs[i % 4][:], AF.Square, accum_out=s_[:, 0:1])
            P.act(s_[:, 1:2], s_[:, 0:1], AF.Sqrt, bias=EPS, scale=1.0 / D)

        def z2(i):
            s_ = sm[i % 4]
            P.recip(s_[:, 1:2], s_[:, 1:2])
            P.stt("dve", os_[i % 3][:], xs[i % 4][:], s_[:, 1:2], fn[:], ALU.mult, ALU.mult)

        def z3(i):
            P.dma("act", out[i * 128:(i + 1) * 128, :], os_[i % 3][:])

        skew([z0, z1, z2, z3], T // 128)
    keep.close()


def host_inputs(inputs, b):
    f = lambda a: np.ascontiguousarray(np.asarray(a), dtype=np.float32)
    m = {}
    m["x"] = f(inputs["x"][b])
    m["ctx"] = f(inputs["ctx"][b])
    cc = np.stack([np.asarray(inputs["c"][b]).reshape(8, 128).T, np.asarray(inputs["c_ctx"]).reshape(8, 128).T], axis=-1)
    m["cc"] = f(cc)
    m["w_mod"] = f(inputs["w_mod"][0])
    m["b_mod"] = f(inputs["b_mod"][0]).reshape(1, -1)
    m["norm_mix"] = f(inputs["norm_mix"][0]).reshape(1, -1)
    m["norm_ffn"] = f(inputs["norm_ffn"][0]).reshape(1, -1)
    m["w_in"] = f(inputs["w_in"][0])
    m["ident_f"] = np.eye(128, dtype=np.float32)
    na_index_tables()
    m.update({k_: v_ for k_, v_ in host_consts().items() if k_ != "na_idx"})
    m["cwl"] = f(np.asarray(inputs["conv_qkv"][0]).reshape(5, 12, 128).transpose(2, 1, 0))
    m["a_log"] = f(inputs["a_log"][0]).reshape(1, 8)
    m["dt_bias"] = f(inputs["dt_bias"][0]).reshape(1, 8)
    m["gdn_norm"] = f(inputs["gdn_norm"][0]).reshape(1, 128)
    m["w_out"] = f(inputs["w_out"][0])
    m["w_router_l"] = f(np.asarray(inputs["w_router"][0]).reshape(8, 128, NE).transpose(1, 0, 2))
    m["final_norm"] = f(inputs["final_norm"]).reshape(1, D)
    m["w_gate"] = f(inputs["w_gate"][0])
    m["w_up"] = f(inputs["w_up"][0])
    m["w_down"] = f(inputs["w_down"][0])
    ridx, cidx = na_index_tables()
    rpb = f(inputs["na_rpb"][0])
    m["na_bias"] = np.ascontiguousarray(rpb[:, ridx, cidx])
    return m


def na_index_tables():
    if "na_idx" in _CONSTS:
        return _CONSTS["na_idx"]
    ridx = np.zeros((128, 21, 128), np.int64)
    cidx = np.zeros((128, 21, 128), np.int64)
    mask = np.full((128, 21, 128), NEG, np.float32)
    key = np.arange(128)
    kr2, kc = key // 64, key % 64
    qr2, qc = key // 64, key % 64
    cs = np.clip(qc - 8, 0, 48)
    for a in (0, 1, 2, 30, 31):
        for (ti, m_) in na_tiles(a):
            krow = (2 * m_ + kr2)[:, None]
            qrow = (2 * a + qr2)[None, :]
            rs = np.clip(qrow - 4, 0, 56)
            vr = (krow >= rs) & (krow < rs + 8)
            vc = (kc[:, None] >= cs[None, :]) & (kc[:, None] < cs[None, :] + 16)
            valid = vr & vc
            ridx[:, ti, :] = np.clip(krow - qrow + 7, 0, 14)
            cidx[:, ti, :] = np.clip(kc[:, None] - qc[None, :] + 15, 0, 30)
            mask[:, ti, :] = np.where(valid, 0.0, NEG)
    _CONSTS["na_idx"] = (ridx, cidx)
    _CONSTS["na_mask"] = mask
    return _CONSTS["na_idx"]


_CONSTS = {}


def host_consts():
    if "cmat" in _CONSTS:
        return _CONSTS
    r = np.arange(128)[:, None]
    c = np.arange(128)[None, :]
    same = (r // 64) == (c // 64)
    cm = np.zeros((128, 8, 128), np.float32)
    cm[:, 0] = same & (r > c)
    cm[:, 1] = same & (r <= c)
    cm[:, 2] = same & (r < c)
    cm[:, 3] = same & (r >= c)
    cm[:, 4] = 1.0
    partner = np.where((np.arange(128) % 64) < 32, np.arange(128) + 32, np.arange(128) - 32)
    cm[partner, 5, np.arange(128)] = 1.0
    cm[:, 6] = (r < 64)
    cm[:, 7] = (r >= 64)
    _CONSTS["cmat"] = cm
    pairs = 32
    inv_freq = (np.float32(10000.0) ** (-np.arange(pairs, dtype=np.float32) / np.float32(pairs))).astype(np.float32)
    t = np.arange(T)
    pos_r = (t // 64).astype(np.float32)
    pos_c = (t % 64).astype(np.float32)
    cos = np.zeros((128, T), np.float32)
    sin = np.zeros((128, T), np.float32)
    for p in range(128):
        j = p % 32
        pos = pos_r if p < 64 else pos_c
        ang = (pos * inv_freq[j]).astype(np.float32)
        cos[p] = np.cos(ang).astype(np.float32)
        sgn = -1.0 if (p % 64) < 32 else 1.0
        sin[p] = sgn * np.sin(ang).astype(np.float32)
    _CONSTS["rope_cos"] = cos
    _CONSTS["rope_sin"] = sin
    return _CONSTS


_NC_CACHE = {}


def kernel(**inputs):
    if "nc" not in _NC_CACHE:
        nc = bass.Bass("TRN2", target_bir_lowering=False)
        P, I = build(nc)
        P.finish()
        _NC_CACHE["nc"] = (nc, sorted(I.keys()))
    nc, names = _NC_CACHE["nc"]
    in_maps = []
    for b in range(NCORES):
        full = host_inputs(inputs, b)
        in_maps.append({k: full[k] for k in names})
    res = run_bass_kernel_spmd(nc, in_maps, core_ids=list(range(NCORES)))
    outs = [np.asarray(res.results[b]["out"], dtype=np.float32) for b in range(NCORES)]
    return np.stack(outs, axis=0)
```

```python
import numpy as np
from contextlib import ExitStack
import concourse.bass as bass
import concourse.mybir as mybir
from concourse.bass_utils import run_bass_kernel_spmd
import ml_dtypes

F32 = mybir.dt.float32
BF16 = mybir.dt.bfloat16
U32 = mybir.dt.uint32
AF = mybir.ActivationFunctionType
ALU = mybir.AluOpType
AX = mybir.AxisListType

D = 1024
T = 4096
TC = 256
TT = T + TC
NCORES = 8
INC = 3600
GW = 512
NE = 16
CAP = 512
EPS = 1e-6
NEG = -30000.0


class Prog:
    ENG = ("pe", "dve", "act", "pool", "sp")

    def __init__(self, nc, n_dma_sems=48):
        self.nc = nc
        self.es = ExitStack()
        self.eng = dict(pe=nc.tensor, dve=nc.vector, act=nc.scalar, pool=nc.gpsimd, sp=nc.sync)
        self.csem = {e: self.es.enter_context(nc.semaphore("cs_" + e)) for e in self.ENG}
        self.ccnt = {e: 0 for e in self.ENG}
        self.dsem = [self.es.enter_context(nc.semaphore("ds%d" % i)) for i in range(n_dma_sems)]
        self.dcnt = [0] * n_dma_sems
        self.dpool = {"sp": list(range(0, 20)), "act": list(range(20, 28)), "pool": list(range(28, n_dma_sems))}
        self.dnext = {"sp": 0, "act": 0, "pool": 0}
        self.seen = {e: {} for e in self.ENG}
        self.recs = {}
        self.ninst = 0
        self.uid = 0

    def sb(self, stack, shape, dt, name=None):
        self.uid += 1
        return stack.enter_context(self.nc.sbuf_tensor("%s_%d" % (name or "t", self.uid), list(shape), dt))

    def ps(self, stack, shape, dt, name=None):
        self.uid += 1
        return stack.enter_context(self.nc.psum_tensor("%s_%d" % (name or "p", self.uid), list(shape), dt))

    def dram(self, name, shape, dt, kind="Internal"):
        if name in getattr(self, "dump", ()):
            kind = "ExternalOutput"
        return self.nc.dram_tensor(name, list(shape), dt, kind=kind).ap()

    def _sem(self, i):
        return self.csem[i[1]] if i[0] == "c" else self.dsem[i[1]]

    def _box(self, ap):
        t = ap.tensor
        pairs = [(int(s), int(c)) for s, c in ap.ap]
        off = int(ap.offset)
        if type(t).__name__.startswith("DRam"):
            ext = sum((c - 1) * abs(s) for s, c in pairs)
            return t.name, (0, 1, off, off + ext + 1)
        shp = [int(v) for v in t.shape]
        if type(t).__name__.startswith("PSum"):
            esz = 2 if ap.dtype == BF16 else 4
            fsz = 1
            for v in shp[1:]:
                fsz *= v
            f0 = off % fsz
            ext = sum((c - 1) * abs(s) for s, c in pairs[1:])
            b0 = (f0 * esz) // 2048
            b1 = ((f0 + ext + 1) * esz - 1) // 2048 + 1
            return "PS!" + t.name, (0, 128, b0 * 2048, b1 * 2048)
        fsz = 1
        for v in shp[1:]:
            fsz *= v
        ps_, pc = pairs[0]
        p0 = off // fsz
        f0 = off % fsz
        if ps_ != fsz:
            if ps_ == 0 or pc == 1:
                pc = 1
            else:
                return t.name, (0, 128, 0, fsz)
        ext = sum((c - 1) * abs(s) for s, c in pairs[1:])
        return t.name, (p0, p0 + pc, f0, f0 + ext + 1)

    def _deps(self, ap, is_write):
        name, (p0, p1, lo, hi) = self._box(ap)
        if name.startswith("PS!"):
            is_write = True
        out = []
        for r in self.recs.get(name, ()):
            if r[0] >= p1 or p0 >= r[1] or r[2] >= hi or lo >= r[3]:
                continue
            if is_write or r[4]:
                out.append(r[5])
        return out

    def _record(self, ap, is_write, ev):
        name, (p0, p1, lo, hi) = self._box(ap)
        if name.startswith("PS!"):
            is_write = True
        lst = self.recs.setdefault(name, [])
        keep = []
        for r in lst:
            covered = r[0] >= p0 and r[1] <= p1 and r[2] >= lo and r[3] <= hi
            if covered and (is_write or ((not r[4]) and r[5][0] == ev[0] and ev[0][0] == "c")):
                continue
            keep.append(r)
        keep.append((p0, p1, lo, hi, is_write, ev))
        self.recs[name] = keep

    def _wait(self, e, evs):
        need = {}
        for (i, v) in evs:
            if e == "pe" and i == ("c", "pe"):
                continue
            if self.seen[e].get(i, 0) < v and need.get(i, 0) < v:
                need[i] = v
        for i, v in need.items():
            self.eng[e].wait_ge(self._sem(i), v)
            self.seen[e][i] = v
            self.ninst += 1

    def _pre(self, e, reads, writes):
        evs = []
        for ap in reads:
            evs += self._deps(ap, False)
        for ap in writes:
            evs += self._deps(ap, True)
        self._wait(e, evs)

    def _post(self, e, ins, reads, writes):
        self.ccnt[e] += 1
        ins.then_inc(self.csem[e], 1)
        ev = (("c", e), self.ccnt[e])
        for ap in reads:
            self._record(ap, False, ev)
        for ap in writes:
            self._record(ap, True, ev)
        self.ninst += 1

    def dma(self, q, out, in_, extra_reads=(), **kw):
        reads = [in_] + list(extra_reads)
        writes = [out]
        evs = []
        for ap in reads:
            evs += self._deps(ap, False)
        for ap in writes:
            evs += self._deps(ap, True)
        slot = self._slot(q)
        if self.dcnt[slot] > 0:
            evs.append((("d", slot), self.dcnt[slot]))
        self._wait(q, evs)
        ins = self.eng[q].dma_start(out=out, in_=in_, **kw)
        self._dma_post(ins, slot, reads, writes)

    def _slot(self, q):
        lst = self.dpool[q]
        slot = lst[self.dnext[q] % len(lst)]
        self.dnext[q] += 1
        return slot

    def _dma_post(self, ins, slot, reads, writes):
        self.dcnt[slot] += 16
        ins.then_inc(self.dsem[slot], 16)
        ev = (("d", slot), self.dcnt[slot])
        for ap in reads:
            self._record(ap, False, ev)
        for ap in writes:
            self._record(ap, True, ev)
        self.ninst += 1

    def gather(self, out, src, idx):
        reads = [src, idx]
        writes = [out]
        evs = []
        for ap in reads:
            evs += self._deps(ap, False)
        evs += self._deps(out, True)
        slot = self._slot("pool")
        if self.dcnt[slot] > 0:
            evs.append((("d", slot), self.dcnt[slot]))
        self._wait("pool", evs)
        ins = self.nc.gpsimd.indirect_dma_start(out=out, out_offset=None, in_=src,
                                                in_offset=bass.IndirectOffsetOnAxis(ap=idx, axis=0))
        self._dma_post(ins, slot, reads, writes)

    def scatter_add(self, dst, src, idx, after=None):
        reads = [src, idx]
        evs = []
        for ap in reads:
            evs += self._deps(ap, False)
        if after is None:
            evs += self._deps(dst, True)
        else:
            evs += list(after)
        slot = self._slot("pool")
        if self.dcnt[slot] > 0:
            evs.append((("d", slot), self.dcnt[slot]))
        self._wait("pool", evs)
        ins = self.nc.gpsimd.indirect_dma_start(out=dst, out_offset=bass.IndirectOffsetOnAxis(ap=idx, axis=0),
                                                in_=src, in_offset=None, compute_op=ALU.add)
        self._dma_post(ins, slot, reads, [dst] if after is None else [])
        return (("d", slot), self.dcnt[slot])

    def mm(self, out, lhsT, rhs, start=True, stop=True):
        self._pre("pe", [lhsT, rhs], [out])
        ins = self.nc.tensor.matmul(out, lhsT=lhsT, rhs=rhs, start=start, stop=stop)
        self._post("pe", ins, [lhsT, rhs], [out])

    def tr(self, out, in_, ident):
        self._pre("pe", [in_, ident], [out])
        ins = self.nc.tensor.transpose(out, in_, ident)
        self._post("pe", ins, [in_, ident], [out])

    def act(self, out, in_, func, bias=None, scale=None, accum_out=None):
        reads = [in_]
        writes = [out]
        kw = {}
        if bias is not None:
            kw["bias"] = bias
            if not isinstance(bias, (int, float)):
                reads.append(bias)
        if scale is not None:
            kw["scale"] = scale
            if not isinstance(scale, (int, float)):
                reads.append(scale)
        if accum_out is not None:
            kw["accum_out"] = accum_out
            writes.append(accum_out)
        self._pre("act", reads, writes)
        ins = self.nc.scalar.activation(out=out, in_=in_, func=func, **kw)
        self._post("act", ins, reads, writes)

    def tt(self, e, out, in0, in1, op):
        self._pre(e, [in0, in1], [out])
        ins = self.eng[e].tensor_tensor(out=out, in0=in0, in1=in1, op=op)
        self._post(e, ins, [in0, in1], [out])

    def ts(self, e, out, in0, s1, s2, op0, op1=None, accum_out=None):
        reads = [in0]
        writes = [out]
        for s in (s1, s2):
            if s is not None and not isinstance(s, (int, float)):
                reads.append(s)
        kw = {}
        if op1 is not None:
            kw["op1"] = op1
        if accum_out is not None:
            kw["accum_out"] = accum_out
            writes.append(accum_out)
        self._pre(e, reads, writes)
        ins = self.eng[e].tensor_scalar(out=out, in0=in0, scalar1=s1, scalar2=s2, op0=op0, **kw)
        self._post(e, ins, reads, writes)

    def stt(self, e, out, in0, scalar, in1, op0, op1):
        reads = [in0, in1]
        if not isinstance(scalar, (int, float)):
            reads.append(scalar)
        assert e == "dve"
        self._pre(e, reads, [out])
        ins = self.eng[e].scalar_tensor_tensor(out=out, in0=in0, scalar=scalar, in1=in1, op0=op0, op1=op1)
        self._post(e, ins, reads, [out])

    def copy(self, e, out, in_):
        self._pre(e, [in_], [out])
        if e == "act":
            ins = self.nc.scalar.copy(out=out, in_=in_)
        else:
            ins = self.eng[e].tensor_copy(out=out, in_=in_)
        self._post(e, ins, [in_], [out])

    def memset(self, e, ap, val):
        self._pre(e, [], [ap])
        ins = self.eng[e].memset(ap, val)
        self._post(e, ins, [], [ap])

    def reduce(self, e, out, in_, op, axis=AX.X):
        self._pre(e, [in_], [out])
        ins = self.eng[e].tensor_reduce(out=out, in_=in_, axis=axis, op=op)
        self._post(e, ins, [in_], [out])

    def recip(self, out, in_):
        self._pre("dve", [in_], [out])
        ins = self.nc.vector.reciprocal(out=out, in_=in_)
        self._post("dve", ins, [in_], [out])

    def rsqrt(self, out, in_, scale=1.0, bias=0.0):
        self.act(out, in_, AF.Sqrt, bias=bias, scale=scale)
        self.recip(out, out)

    def max8(self, out, in_):
        self._pre("dve", [in_], [out])
        ins = self.nc.vector.max(out=out, in_=in_)
        self._post("dve", ins, [in_], [out])

    def max_index(self, out, in_max, in_values):
        self._pre("dve", [in_max, in_values], [out])
        ins = self.nc.vector.max_index(out=out, in_max=in_max, in_values=in_values)
        self._post("dve", ins, [in_max, in_values], [out])

    def match_replace(self, out, in_to_replace, in_values, imm):
        self._pre("dve", [in_to_replace, in_values], [out])
        ins = self.nc.vector.match_replace(out=out, in_to_replace=in_to_replace, in_values=in_values, imm_value=imm)
        self._post("dve", ins, [in_to_replace, in_values], [out])

    def barrier(self):
        evs = [(("c", e), self.ccnt[e]) for e in self.ENG if self.ccnt[e] > 0]
        evs += [(("d", i), self.dcnt[i]) for i in range(len(self.dsem)) if self.dcnt[i] > 0]
        for e in self.ENG:
            self._wait(e, [ev for ev in evs if ev[0] != ("c", e)])
        self.recs = {}

    def finish(self):
        evs = [(("c", e), self.ccnt[e]) for e in self.ENG if self.ccnt[e] > 0 and e != "sp"]
        evs += [(("d", i), self.dcnt[i]) for i in range(len(self.dsem)) if self.dcnt[i] > 0]
        self._wait("sp", evs)
        self.es.close()


def skew(phases, n):
    for it in range(n + len(phases) - 1):
        for k, ph in enumerate(phases):
            i = it - k
            if 0 <= i < n:
                ph(i)


def bcast_rows(ap_row, nparts):
    pairs = [(int(s), int(c)) for s, c in ap_row.ap]
    last = pairs[-1]
    return bass.AP(tensor=ap_row.tensor, offset=int(ap_row.offset), ap=[[0, nparts], [last[0], last[1]]])


def build(nc, upto=99, dbg=None):
    P = Prog(nc)
    dbg = dbg or {}
    P.dump = set(dbg.get("dump", ()))
    I = {}
    inp = lambda n, s, dt=F32: I.setdefault(n, nc.dram_tensor(n, list(s), dt, kind="ExternalInput").ap())
    x = inp("x", [T, D])
    ctx = inp("ctx", [TC, D])
    cc = inp("cc", [128, 8, 2])
    w_mod = inp("w_mod", [D, 6 * D])
    b_mod = inp("b_mod", [1, 6 * D])
    norm_mix = inp("norm_mix", [1, D])
    norm_ffn = inp("norm_ffn", [1, D])
    w_in = inp("w_in", [D, INC])
    ident_f = inp("ident_f", [128, 128])
    out = nc.dram_tensor("out", [T, D], F32, kind="ExternalOutput").ap()

    S_mod = P.dram("S_mod", [2, 6 * D], F32)
    S_qkvT = P.dram("S_qkvT", [3 * GW, TT], BF16)
    S_gate = P.dram("S_gate", [T, GW], F32)
    S_ab = P.dram("S_ab", [TT, 16], F32)
    S_naqT = P.dram("S_naqT", [8, 64, T], BF16)
    S_nakT = P.dram("S_nakT", [8, 64, TT], BF16)
    S_nav = P.dram("S_nav", [TT, GW], BF16)
    S_ymix = P.dram("S_ymix", [T, D], BF16)

    glob = ExitStack()
    identf = P.sb(glob, [128, 128], F32, "identf")
    identb = P.sb(glob, [128, 128], BF16, "identb")
    P.dma("sp", identf[:], ident_f[:, :])
    P.copy("dve", identb[:], identf[:])

    wstk = ExitStack()
    wsb = P.sb(wstk, [128, 8, INC], BF16, "wsb")
    for kc in range(8):
        for hf in range(2):
            P.dma("pool", wsb[:, kc, hf * 1800:(hf + 1) * 1800], w_in[kc * 128:(kc + 1) * 128, hf * 1800:(hf + 1) * 1800])
    with ExitStack() as st:
        cct = P.sb(st, [128, 8, 2], F32, "cct")
        sct = P.sb(st, [128, 8, 2], F32, "sct")
        bm = P.sb(st, [2, 6 * D], F32, "bm")
        mrow = P.sb(st, [2, 6 * D], F32, "mrow")
        wm = [P.sb(st, [128, 3 * D], F32, "wm") for _ in range(3)]
        mps = P.ps(st, [128, 3 * D], F32, "mps")
        P.dma("sp", cct[:], cc[:, :, :])
        P.dma("sp", bm[0:1, :], b_mod[:, :])
        P.dma("sp", bm[1:2, :], b_mod[:, :])
        P.act(sct[:], cct[:], AF.Silu)
        it = 0
        for nh in range(2):
            for kc in range(8):
                w = wm[it % 3]
                it += 1
                P.dma("sp" if it % 2 else "act", w[:], w_mod[kc * 128:(kc + 1) * 128, nh * 3 * D:(nh + 1) * 3 * D])
                for nb in range(6):
                    P.mm(mps[0:2, nb * 512:(nb + 1) * 512], sct[:, kc, :], w[:, nb * 512:(nb + 1) * 512],
                         start=(kc == 0), stop=(kc == 7))
            P.tt("dve", mrow[:, nh * 3 * D:(nh + 1) * 3 * D], mps[0:2, :], bm[:, nh * 3 * D:(nh + 1) * 3 * D], ALU.add)
        P.dma("sp", S_mod[:, :], mrow[:])
    P.barrier()
    if "mod" in dbg:
        return P, I

    bc = ExitStack()

    def bc_tile(row_ap, name):
        t = P.sb(bc, [128, D], F32, name)
        P.dma("sp", t[:], bcast_rows(row_ap, 128))
        return t

    sh1 = bc_tile(S_mod[0:1, 0:D], "sh1")
    gm1 = bc_tile(S_mod[0:1, D:2 * D], "gm1")
    sh1c = bc_tile(S_mod[1:2, 0:D], "sh1c")
    gm1c = bc_tile(S_mod[1:2, D:2 * D], "gm1c")
    nmx = bc_tile(norm_mix[0:1, :], "nmx")
    for g_ in (gm1, gm1c):
        P.stt("dve", g_[:], g_[:], 1.0, nmx[:], ALU.add, ALU.mult)

    with ExitStack() as st:
        xts = [P.sb(st, [128, D], F32, "xt") for _ in range(3)]
        junk = P.sb(st, [128, D], BF16, "junk")
        tmps = [P.sb(st, [128, D], F32, "tmp") for _ in range(2)]
        hbs = [P.sb(st, [128, D], BF16, "hb") for _ in range(2)]
        hTs = [P.sb(st, [128, 8, 512], BF16, "hT") for _ in range(2)]
        sss = [P.sb(st, [128, 2], F32, "ss") for _ in range(4)]
        stf = [P.sb(st, [128, 512], F32, "stf") for _ in range(4)]
        stb = [P.sb(st, [128, 512], BF16, "stb") for _ in range(4)]
        trp = [P.ps(st, [128, 8, 128], BF16, "trp") for _ in range(2)]
        pps = [P.ps(st, [128, 512], F32, "pps") for _ in range(4)]
        cnt = dict(x=0, t=0, p=0, sf=0, sb=0, ev=0)

        def evac(dst, src):
            cnt["ev"] += 1
            if cnt["ev"] % 2:
                P.act(dst, src, AF.Copy)
            else:
                P.copy("dve", dst, src)

        blocks = [(src, b0, 512) for src in ("x",) for b0 in range(0, T, 512)] + [("c", 0, 256)]
        hb8 = hbs + [P.sb(st, [128, D], BF16, "hb") for _ in range(6)]

        def prep_elem(bi):
            (srcn, b0, nt) = blocks[bi]
            src = x if srcn == "x" else ctx
            gmt, sht = (gm1, sh1) if srcn == "x" else (gm1c, sh1c)
            for i in range(nt // 128):
                xt = xts[cnt["x"] % 3]
                ss = sss[cnt["x"] % 4]
                tmp = tmps[cnt["x"] % 2]
                hb = hb8[(bi % 2) * 4 + i]
                cnt["x"] += 1
                P.dma("sp", xt[:], src[b0 + i * 128:b0 + (i + 1) * 128, :])
                P.act(junk[:], xt[:], AF.Square, accum_out=ss[:, 0:1])
                P.rsqrt(ss[:, 1:2], ss[:, 0:1], scale=1.0 / D, bias=EPS)
                P.stt("dve", tmp[:], xt[:], ss[:, 1:2], gmt[:], ALU.mult, ALU.mult)
                P.tt("pool", hb[:], tmp[:], sht[:], ALU.add)

        def prep_tr(bi):
            (srcn, b0, nt) = blocks[bi]
            hT = hTs[bi % 2]
            for i in range(nt // 128):
                hb = hb8[(bi % 2) * 4 + i]
                tp = trp[i % 2]
                for kc in range(8):
                    P.tr(tp[:, kc, :], hb[:, kc * 128:(kc + 1) * 128], identb[:])
                evac(hT[:, :, i * 128:(i + 1) * 128], tp[:, :, :])

        def mms(bi):
            (srcn, b0, nt) = blocks[bi]
            tok0 = b0 if srcn == "x" else T + b0
            hT = hTs[bi % 2]
            fm = [("qkv", c_) for c_ in range(12)] + ([("naq", c_) for c_ in range(4)] if srcn == "x" else []) + \
                 [("nak", c_) for c_ in range(4)]
            for (kind, c_) in fm:
                col0 = {"qkv": 0, "naq": 2064, "nak": 2064 + 512}[kind] + c_ * 128
                pp = pps[cnt["p"] % 4]
                cnt["p"] += 1
                for kc in range(8):
                    P.mm(pp[:, 0:nt], wsb[:, kc, col0:col0 + 128], hT[:, kc, 0:nt], start=(kc == 0), stop=(kc == 7))
                if kind == "qkv":
                    s_ = stb[cnt["sb"] % 4]
                    cnt["sb"] += 1
                    evac(s_[:, 0:nt], pp[:, 0:nt])
                    P.dma("sp", S_qkvT[c_ * 128:(c_ + 1) * 128, tok0:tok0 + nt], s_[:, 0:nt])
                else:
                    s_ = stb[cnt["sb"] % 4]
                    cnt["sb"] += 1
                    if kind == "naq":
                        P.act(s_[:, 0:nt], pp[:, 0:nt], AF.Copy, scale=0.125)
                    else:
                        evac(s_[:, 0:nt], pp[:, 0:nt])
                    dstT = S_naqT if kind == "naq" else S_nakT
                    for hh in range(2):
                        P.dma("sp", dstT[c_ * 2 + hh, :, tok0:tok0 + nt], s_[hh * 64:(hh + 1) * 64, 0:nt])
            for i in range(nt // 128):
                r0 = tok0 + i * 128
                groups = [("nav", 2064 + 1024, 512), ("ab", 2048, 16)] + ([("gate", 1536, 512)] if srcn == "x" else [])
                for (kind, col0, ncol) in groups:
                    pp = pps[cnt["p"] % 4]
                    cnt["p"] += 1
                    for kc in range(8):
                        P.mm(pp[:, 0:ncol], hT[:, kc, i * 128:(i + 1) * 128], wsb[:, kc, col0:col0 + ncol],
                             start=(kc == 0), stop=(kc == 7))
                    if kind == "nav":
                        s_ = stb[cnt["sb"] % 4]
                        cnt["sb"] += 1
                        evac(s_[:, 0:ncol], pp[:, 0:ncol])
                        P.dma("sp", S_nav[r0:r0 + 128, :], s_[:, 0:ncol])
                    else:
                        s_ = stf[cnt["sf"] % 4]
                        cnt["sf"] += 1
                        evac(s_[:, 0:ncol], pp[:, 0:ncol])
                        if kind == "ab":
                            P.dma("sp", S_ab[r0:r0 + 128, :], s_[:, 0:16])
                        else:
                            P.dma("sp", S_gate[r0:r0 + 128, :], s_[:, 0:512])

        prep_elem(0)
        prep_tr(0)
        for bi in range(len(blocks)):
            if bi + 1 < len(blocks):
                prep_elem(bi + 1)
            mms(bi)
            if bi + 1 < len(blocks):
                prep_tr(bi + 1)
    P.barrier()
    bc.close()
    wstk.close()
    if "proj" in dbg:
        return P, I
    build_gdn(P, nc, I, dbg, S_qkvT, S_gate, S_ab, S_ymix, identf, identb)
    if "gdn" in dbg:
        return P, I
    build_na(P, nc, I, dbg, S_naqT, S_nakT, S_nav, S_ymix)
    if "na" in dbg:
        return P, I
    build_tail(P, nc, I, dbg, x, out, S_mod, S_ymix, identf, identb)
    return P, I


def ap3(ap2d, mid=None, last=None):
    pairs = [(int(s_), int(c_)) for s_, c_ in ap2d.ap]
    assert len(pairs) == 2
    if mid is not None:
        ap = [list(pairs[0]), [0, mid], list(pairs[1])]
    else:
        ap = [list(pairs[0]), list(pairs[1]), [0, last]]
    return bass.AP(tensor=ap2d.tensor, offset=int(ap2d.offset), ap=ap)


NDT = F32


def build_gdn(P, nc, I, dbg, S_qkvT, S_gate, S_ab, S_ymix, identf, identb):
    inp = lambda n, s_, dt=F32: I.setdefault(n, nc.dram_tensor(n, list(s_), dt, kind="ExternalInput").ap())
    cmat_d = inp("cmat", [128, 8, 128])
    cwl_d = inp("cwl", [128, 12, 5])
    alog_d = inp("a_log", [1, 8])
    dtb_d = inp("dt_bias", [1, 8])
    gnorm_d = inp("gdn_norm", [1, 128])
    cos_d = inp("rope_cos", [128, T])
    sin_d = inp("rope_sin", [128, T])
    NP = TT // 128
    with ExitStack() as st:
        cm = P.sb(st, [128, 8, 128], F32, "cm")
        P.dma("sp", cm[:], cmat_d[:, :, :])
        Sm = [cm[:, 0, :], cm[:, 2, :]]
        Im = [cm[:, 1, :], cm[:, 3, :]]
        ones = cm[:, 4, :]
        perm = cm[:, 5, :]
        indA = cm[:, 6, :]
        indB = cm[:, 7, :]
        negm = P.sb(st, [128, 4, 128], BF16, "negm")
        for k_ in range(4):
            P.ts("dve", negm[:, k_, :], cm[:, k_, :], -1.0, -NEG, ALU.add, ALU.mult)
        negS = [negm[:, 0, :], negm[:, 2, :]]
        negI = [negm[:, 1, :], negm[:, 3, :]]
        posm = P.sb(st, [128, 2, 128], BF16, "posm")
        for k_, src_ in enumerate((0, 2)):
            P.ts("dve", posm[:, k_, :], cm[:, src_, :], -1.0, NEG, ALU.add, ALU.mult)
        posS = [posm[:, 0, :], posm[:, 1, :]]
        ones_b = P.sb(st, [128, 128], BF16, "ones_b")
        P.memset("pool", ones_b[:], 1.0)
        cwl = P.sb(st, [128, 12, 5], F32, "cwl")
        P.dma("sp", cwl[:], cwl_d[:, :, :])
        gnb = P.sb(st, [128, 128], F32, "gnb")
        P.dma("sp", gnb[:], bcast_rows(gnorm_d[0:1, :], 128))
        negA = P.sb(st, [128, 8], F32, "negA")
        dtb = P.sb(st, [128, 8], F32, "dtb")
        P.dma("sp", negA[:], bcast_rows(alog_d[0:1, :], 128))
        P.dma("sp", dtb[:], bcast_rows(dtb_d[0:1, :], 128))
        P.act(negA[:], negA[:], AF.Exp)
        P.ts("dve", negA[:], negA[:], -1.0, None, ALU.mult)
        banks = [P.ps(st, [128, 4, 128], F32, "gb") for _ in range(7)]
        trb = P.ps(st, [128, 8, 128], BF16, "gtr")
        cnt = dict(j=0, s=0, rr=0)

        def newbank():
            cnt["j"] += 1
            return banks[cnt["j"] % 4]

        ab_t = P.sb(st, [128, NP, 16], F32, "ab_t")
        P.dma("sp", ab_t[:], S_ab.rearrange("(n p) c -> p n c", p=128))
        g_t = P.sb(st, [128, NP, 8], F32, "g_t")
        nbeta_t = P.sb(st, [128, NP, 8], F32, "nbeta_t")
        beta_t = P.sb(st, [128, NP, 8], F32, "beta_t")
        bG_t = P.sb(st, [128, NP, 8], F32, "bG_t")
        Erem_t = P.sb(st, [128, NP, 8], F32, "Erem_t")
        Egl_t = P.sb(st, [128, 2, NP, 8], F32, "Egl_t")
        gc_t = P.sb(st, [128, NP, 8], F32, "gc_t")
        ngc_t = P.sb(st, [128, NP, 8], F32, "ngc_t")
        ghb_t = P.sb(st, [128, NP, 8], BF16, "ghb_t")
        ghf_t = P.sb(st, [128, NP, 8], F32, "ghf_t")
        glf_t = P.sb(st, [128, NP, 8], F32, "glf_t")
        P.tt("dve", g_t[:], ab_t[:, :, 0:8], ap3(dtb[:, :], mid=NP), ALU.add)
        P.act(g_t[:], g_t[:], AF.Exp)
        P.act(g_t[:], g_t[:], AF.Ln, bias=1.0, scale=1.0)
        P.tt("dve", g_t[:], g_t[:], ap3(negA[:, :], mid=NP), ALU.mult)
        P.act(beta_t[:], ab_t[:, :, 8:16], AF.Sigmoid)
        P.ts("dve", nbeta_t[:], beta_t[:], -1.0, None, ALU.mult)
        for d in range(2):
            for kind, mat, dst in (("cum", Im[d], bG_t), ("rem", Sm[d], Erem_t)):
                pq = banks[0]
                P.mm(pq[:, :, :].rearrange("p a b -> p (a b)")[:, 0:NP * 4].rearrange("p (n c) -> p n c", c=4),
                     mat, g_t[:, :, d * 4:(d + 1) * 4])
                pv_ = pq[:, :, :].rearrange("p a b -> p (a b)")[:, 0:NP * 4].rearrange("p (n c) -> p n c", c=4)
                if kind == "cum":
                    P.copy("dve", gc_t[:, :, d * 4:(d + 1) * 4], pv_)
                P.act(dst[:, :, d * 4:(d + 1) * 4], pv_, AF.Exp)
        for c_, ind in enumerate((indA, indB)):
            pq = banks[1]
            v = pq[:, :, :].rearrange("p a b -> p (a b)")[:, 0:NP * 8].rearrange("p (n c) -> p n c", c=8)
            P.mm(v, ind, g_t[:, :, :])
            P.act(Egl_t[:, c_, :, :], v, AF.Exp)
        P.ts("dve", ngc_t[:], gc_t[:], -1.0, None, ALU.mult)
        P.copy("dve", ghb_t[:], g_t[:])
        P.copy("dve", ghf_t[:], ghb_t[:])
        P.tt("dve", glf_t[:], g_t[:], ghf_t[:], ALU.subtract)
        P.copy("dve", ghb_t[:], glf_t[:])
        P.copy("dve", glf_t[:], ghb_t[:])
        P.tt("dve", bG_t[:], bG_t[:], beta_t[:], ALU.mult)
        if "gdnA" in dbg:
            for nm, t_ in (("D_g", g_t), ("D_beta", beta_t), ("D_bG", bG_t), ("D_Erem", Erem_t)):
                o_ = P.dram(nm, [128, NP, 8], F32)
                P.dma("sp", o_[:, :, :], t_[:])
            o_ = P.dram("D_Egl", [128, 2, NP, 8], F32)
            P.dma("sp", o_[:, :, :, :], Egl_t[:])
            return

        p_bs = [P.sb(st, [128, TT], BF16, "p_b") for _ in range(2)]
        diagws = [P.sb(st, [128, 5, 128], BF16, "diagw") for _ in range(2)]
        svq = [P.sb(st, [128, 512], F32, "svq") for _ in range(4)]
        svb = P.sb(st, [128, TT], BF16, "svb")
        qT = P.sb(st, [128, TT], BF16, "qT")
        kT = P.sb(st, [128, TT], BF16, "kT")
        k_tok = P.sb(st, [128, NP, 128], BF16, "k_tok")
        v_tok = P.sb(st, [128, NP, 128], BF16, "v_tok")
        o_acc = P.sb(st, [128, 32, 128], F32, "o_acc")
        gate_s = [P.sb(st, [128, 8, 128], F32, "gate_s") for _ in range(2)]
        ssq_s = [P.sb(st, [128, 8, 128], F32, "ssq_s") for _ in range(2)]
        blk = {n_: [P.sb(st, [128, 512], F32, n_) for _ in range(2)] for n_ in ("sqb", "rs", "qn", "t1", "t2", "cosb", "sinb")}
        RING = 4
        ring = {}
        for d in range(2):
            for r_ in range(RING):
                ring[(d, r_)] = dict(
                    wT=P.sb(st, [128, 128], BF16, "wT"), u=P.sb(st, [128, 128], F32, "u"),
                    attnT=P.sb(st, [128, 128], BF16, "attnT"), qdT=P.sb(st, [128, 128], BF16, "qdT"),
                    kdec=P.sb(st, [128, 128], BF16, "kdec"))
        NJ = 4
        jrg = [{n_: P.sb(st, [128, 128], BF16, n_) for n_ in ("rhsGh", "rhsGl")} for _ in range(4)]
        jt = [{n_: P.sb(st, [128, 128], F32, n_) for n_ in ("E", "ET", "Gbc")}
              for _ in range(NJ)]
        jtb = [{n_: P.sb(st, [128, 128], BF16, n_) for n_ in ("TTb", "vb", "kbd", "Qb", "Q2b")}
               for _ in range(NJ)]
        jU = [[P.sb(st, [128, 4, 128], BF16, "jU") for _ in range(2)] for _ in range(NJ)]
        for js_ in range(NJ):
            for u_ in jU[js_]:
                P.memset("pool", u_[:, 1, :], 0.0)
        Sst = [P.sb(st, [128, 128], F32, "Sst") for _ in range(2)]
        Sbf = [P.sb(st, [128, 128], BF16, "Sbf") for _ in range(2)]
        vn = [[P.sb(st, [128, 128], BF16, "vn") for _ in range(2)] for _ in range(2)]
        for d in range(2):
            for c_ in range(2):
                P.memset("pool", vn[d][c_][:], 0.0)
        rsn = P.sb(st, [128, 32], F32, "rsn")
        ybfs = [P.sb(st, [128, 8, 128], BF16, "ybf") for _ in range(2)]
        pending_epi = []

        def tokoff(n):
            return n * 128 if n < 32 else T + (n - 32) * 128

        def job_levels(h, d, n, slot, js):
            dh = d * 4 + h
            t0 = tokoff(n)
            J = jt[js]
            Jb = jtb[js]
            R = ring[(d, slot)]
            lat = n < 32
            gcol = g_t[:, n, dh:dh + 1]
            lv = []
            bk = {}
            U = jU[js]

            def a0():
                P.act(jrg[js]["rhsGh"][:], Im[d], AF.Copy, scale=ghf_t[:, n, dh:dh + 1])
                P.act(jrg[js]["rhsGl"][:], Im[d], AF.Copy, scale=glf_t[:, n, dh:dh + 1])
                P.tt("pool", Jb["vb"][:], v_tok[:, n, :], beta_t[:, n, dh:dh + 1].to_broadcast([128, 128]), ALU.mult)

            def a1():
                bA = newbank()
                bk["A"] = bA
                nq = 3 if lat else 1
                P.mm(bA[:, 0:nq, :], ones_b[:], ap3(jrg[js]["rhsGh"][:, :], mid=nq), start=True, stop=False)
                P.mm(bA[:, 0:nq, :], ones_b[:], ap3(jrg[js]["rhsGl"][:, :], mid=nq), start=False, stop=False)
                P.mm(bA[:, 0, :], identb[:], posS[d], start=False, stop=not lat)
                if lat:
                    P.mm(bA[:, 1, :], identb[:], negI[d], start=False, stop=True)
                P.mm(bA[:, 3, :], kT[:, t0:t0 + 128], kT[:, t0:t0 + 128])

            def a2():
                bA = bk["A"]
                P.act(J["E"][:], bA[:, 0, :], AF.Exp, bias=gc_t[:, n, dh:dh + 1], scale=-1.0)
                if lat:
                    P.act(J["ET"][:], bA[:, 1, :], AF.Exp, bias=ngc_t[:, n, dh:dh + 1], scale=1.0)
                    P.act(J["Gbc"][:], bA[:, 2, :], AF.Exp)
                P.tt("pool", Jb["kbd"][:], k_tok[:, n, :], bG_t[:, n, dh:dh + 1].to_broadcast([128, 128]), ALU.mult)

            def a3():
                P.stt("dve", Jb["Qb"][:], bk["A"][:, 3, :], nbeta_t[:, n, dh:dh + 1], J["E"][:], ALU.mult, ALU.mult)
                P.tt("pool", R["kdec"][:], k_tok[:, n, :], Erem_t[:, n, dh:dh + 1].to_broadcast([128, 128]), ALU.mult)

            def a4():
                bB = newbank()
                bk["B"] = bB
                P.tr(trb[:, js, :], Jb["Qb"][:], identb[:])
                if lat:
                    P.mm(bB[:, 0, :], kT[:, t0:t0 + 128], qT[:, t0:t0 + 128])

            def a5():
                P.copy("act", U[0][:, 2, :], trb[:, js, :])
                if lat:
                    P.tt("dve", R["attnT"][:], bk["B"][:, 0, :], J["ET"][:], ALU.mult)
                    P.tt("pool", R["qdT"][:], qT[:, t0:t0 + 128], J["Gbc"][:], ALU.mult)
            lv += [a0, a1, a2, a3, a4, a5]
            Qs = [Jb["Qb"], Jb["Q2b"]]
            for n_ in range(0, 6):
                Uc, Un = U[n_ % 2], U[(n_ + 1) % 2]
                Qc, Qn = Qs[n_ % 2], Qs[(n_ + 1) % 2]

                def m1(n_=n_, Uc=Uc, Qc=Qc):
                    bC = newbank()
                    bk[n_] = bC
                    if n_ < 5:
                        P.mm(bC[:, 0, :], Uc[:, 2, :], Qc[:])
                    if n_ == 0:
                        P.mm(bC[:, 2, :], Qc[:], Uc[:, 2, :])
                    elif n_ < 5:
                        P.mm(bC[:, 1:3, :], Qc[:], Uc[:, 0:3:2, :])
                    else:
                        P.mm(bC[:, 1, :], Qc[:], Uc[:, 0, :])

                def m2(n_=n_, Uc=Uc, Un=Un, Qn=Qn):
                    bC = bk[n_]
                    if n_ < 5:
                        P.copy("act", Qn[:], bC[:, 0, :])
                    if n_ == 0:
                        P.copy("act", Un[:, 2, :], bC[:, 2, :])
                        P.tt("pool", Un[:, 0, :], Uc[:, 2, :], identb[:], ALU.add)
                    elif n_ < 5:
                        P.tt("dve", Un[:, 0:3:2, :], bC[:, 1:3, :], Uc[:, 0:2, :], ALU.add)
                    else:
                        P.tt("dve", Jb["TTb"][:], bC[:, 1, :], Uc[:, 0, :], ALU.add)
                lv += [m1, m2]

            def n1():
                bD = newbank()
                bk["D"] = bD
                P.mm(bD[:, 0, :], Jb["TTb"][:], Jb["vb"][:])
                P.mm(bD[:, 1, :], Jb["kbd"][:], Jb["TTb"][:])

            def n2():
                P.copy("act", R["u"][:], bk["D"][:, 0, :])
                P.copy("act", R["wT"][:], bk["D"][:, 1, :])
            lv += [n1, n2]
            return lv

        def scan_levels(h, d, n, slot):
            dh = d * 4 + h
            R = ring[(d, slot)]
            lat = n < 32
            order = (0, 1) if d == 0 else (1, 0)
            lv = []
            for c_ in order:
                hs = slice(c_ * 64, (c_ + 1) * 64)
                p1, p3, p2 = banks[4 + d][:, 0, :], banks[4 + d][:, 1, :], banks[6][:, d, :]

                def A1(p1=p1):
                    P.mm(p1, R["wT"][:], Sbf[d][:])

                def A2(c_=c_, hs=hs, p1=p1):
                    P.tt("dve", vn[d][c_][hs, :], R["u"][hs, :], p1[hs, :], ALU.subtract)

                def B1(c_=c_, p2=p2, p3=p3):
                    P.mm(p3, R["kdec"][:], vn[d][c_][:])
                    if lat:
                        P.mm(p2, R["qdT"][:], Sbf[d][:], start=True, stop=False)
                        P.mm(p2, R["attnT"][:], vn[d][c_][:], start=False, stop=True)

                def B2(c_=c_, p3=p3):
                    P.stt("dve", Sst[d][:], Sst[d][:], Egl_t[:, c_, n, dh:dh + 1], p3, ALU.mult, ALU.add)

                def B3(hs=hs, p2=p2):
                    P.copy("act", Sbf[d][:], Sst[d][:])
                    if lat:
                        first = (n < 16) if d == 0 else (n >= 16)
                        if first:
                            P.copy("act", o_acc[hs, n, :], p2[hs, :])
                        else:
                            P.tt("dve", o_acc[hs, n, :], o_acc[hs, n, :], p2[hs, :], ALU.add)
                lv += [A1, A2, B1, B2, B3]
            return lv

        heads = dbg.get("gdn_heads", range(4))
        for h in heads:
            kinds = (2, 0, 1)
            blist = [(b0_, 512, 0, T) for b0_ in range(0, T, 512)] + [(T, TC, T, TC)]
            items = [(ki, bi) for ki in range(3) for bi in range(len(blist))]

            def load_kind(ki):
                c_ = kinds[ki] * 4 + h
                P.dma("sp", p_bs[ki % 2][:], S_qkvT[c_ * 128:(c_ + 1) * 128, :])
                for w in range(5):
                    P.act(diagws[ki % 2][:, w, :], identf[:], AF.Copy, scale=cwl[:, c_, w:w + 1])

            def bank512(k_):
                return banks[k_][:, :, :].rearrange("p a b -> p (a b)")

            def f0(i):
                ki, bi = items[i]
                if i == 0:
                    load_kind(0)
                if bi == 3 and ki + 1 < 3:
                    load_kind(ki + 1)
                b0, nb, seq0, L = blist[bi]
                p_b, dg = p_bs[ki % 2], diagws[ki % 2]
                pc = bank512(i % 2)
                P.mm(pc[:, 0:nb], dg[:, 2, :], p_b[:, b0:b0 + nb], start=True, stop=False)
                for w in (0, 1, 3, 4):
                    sh = w - 2
                    j_lo = max(0, seq0 - (b0 + sh))
                    j_hi = min(nb, seq0 + L - (b0 + sh))
                    P.mm(pc[:, j_lo:j_hi], dg[:, w, :], p_b[:, b0 + j_lo + sh:b0 + j_hi + sh], start=False, stop=(w == 4))

            def f1(i):
                ki, bi = items[i]
                b0, nb, seq0, L = blist[bi]
                pc = bank512(i % 2)
                if kinds[ki] == 2:
                    P.act(svb[:, b0:b0 + nb], pc[:, 0:nb], AF.Silu)
                    return
                P.act(svq[i % 4][:, 0:nb], pc[:, 0:nb], AF.Silu)
                P.act(blk["sqb"][i % 2][:, 0:nb], svq[i % 4][:, 0:nb], AF.Square)

            def f2a(i):
                ki, bi = items[i]
                b0, nb, seq0, L = blist[bi]
                if kinds[ki] == 2:
                    return
                pss = bank512(2 + i % 2)
                P.mm(pss[:, 0:nb], ones, blk["sqb"][i % 2][:, 0:nb])

            def f2b(i):
                ki, bi = items[i]
                b0, nb, seq0, L = blist[bi]
                if kinds[ki] == 2:
                    return
                pss = bank512(2 + i % 2)
                P.act(blk["rs"][i % 2][:, 0:nb], pss[:, 0:nb], AF.Sqrt, bias=EPS, scale=1.0)

            def f3a(i):
                ki, bi = items[i]
                b0, nb, seq0, L = blist[bi]
                if kinds[ki] == 2:
                    return
                dstT = qT if kinds[ki] == 0 else kT
                scale = 128 ** -0.5 if kinds[ki] == 0 else 1.0
                rs = blk["rs"][i % 2]
                P.recip(rs[:, 0:nb], rs[:, 0:nb])
                if b0 < T:
                    if not dbg.get("nocs"):
                        P.dma("sp", blk["cosb"][i % 2][:], cos_d[:, b0:b0 + 512])
                    P.stt("dve", blk["qn"][i % 2][:, 0:nb], svq[i % 4][:, 0:nb], scale, rs[:, 0:nb], ALU.mult, ALU.mult)
                else:
                    P.stt("dve", dstT[:, b0:b0 + nb], svq[i % 4][:, 0:nb], scale, rs[:, 0:nb], ALU.mult, ALU.mult)

            def f3b(i):
                ki, bi = items[i]
                b0, nb, seq0, L = blist[bi]
                if kinds[ki] == 2 or b0 >= T:
                    return
                qn = blk["qn"][i % 2]
                psr = bank512(4 + i % 2)
                P.mm(psr[:, 0:nb], perm, qn[:, 0:nb])
                P.tt("pool", blk["t1"][i % 2][:, 0:nb], qn[:, 0:nb], blk["cosb"][i % 2][:, 0:nb], ALU.mult)
                if not dbg.get("nocs"):
                    P.dma("sp", blk["sinb"][i % 2][:], sin_d[:, b0:b0 + 512])

            def f4(i):
                ki, bi = items[i]
                b0, nb, seq0, L = blist[bi]
                if kinds[ki] == 2 or b0 >= T:
                    return
                dstT = qT if kinds[ki] == 0 else kT
                psr = bank512(4 + i % 2)
                P.tt("dve", blk["t2"][i % 2][:, 0:nb], psr[:, 0:nb], blk["sinb"][i % 2][:, 0:nb], ALU.mult)
                P.tt("pool", dstT[:, b0:b0 + nb], blk["t1"][i % 2][:, 0:nb], blk["t2"][i % 2][:, 0:nb], ALU.add)

            phs_ = [f0, f1, f2a, f2b, f3a, f3b, f4]
            for it_ in range(len(items) + len(phs_) - 1):
                for k_, ph_ in enumerate(phs_):
                    i_ = it_ - k_
                    if 0 <= i_ < len(items):
                        ph_(i_)
                if pending_epi and it_ % 6 == 2:
                    pending_epi.pop(0)()
            while pending_epi:
                pending_epi.pop(0)()
            for (srcT, dst_tok) in ((svb, v_tok), (kT, k_tok)):
                for g0 in range(0, NP, 8):
                    ng = min(8, NP - g0)
                    for i_ in range(ng):
                        P.tr(trb[:, i_, :], srcT[:, (g0 + i_) * 128:(g0 + i_ + 1) * 128], identb[:])
                    P.copy("act", dst_tok[:, g0:g0 + ng, :], trb[:, 0:ng, :])
            if "gdnC" in dbg:
                for nm, t_, shp in (("D_qT", qT, [128, TT]), ("D_kT", kT, [128, TT]), ("D_vtok", v_tok, [128, NP, 128]),
                                    ("D_ktok", k_tok, [128, NP, 128])):
                    o_ = P.dram(nm, shp, BF16)
                    P.dma("sp", o_, t_[:])
                return
            for d in range(2):
                P.memset("pool", Sst[d][:], 0.0)
                P.memset("pool", Sbf[d][:], 0.0)
            seq = [[32, 33] + list(range(32)), [33, 32] + list(range(31, -1, -1))]
            prog = dict(jobs_done=set(), scan_done=-1)
            STAG = dbg.get("gdn_stagger", 2)

            def job_stream(par, delay):
                for _ in range(delay):
                    yield
                for s_ in range(par, NP, 2):
                    lvs = [job_levels(h, d, seq[d][s_], s_ % RING, par * 2 + d) for d in range(2)]
                    for li in range(len(lvs[0])):
                        for l_ in lvs:
                            if not dbg.get("gdn_nojobs"):
                                l_[li]()
                        yield
                    prog["jobs_done"].add(s_)

            def scan_stream():
                for s_ in range(NP):
                    while s_ not in prog["jobs_done"]:
                        yield
                    sl = [scan_levels(h, d, seq[d][s_], s_ % RING) for d in range(2)]
                    for li in range(len(sl[0])):
                        for l_ in sl:
                            if not dbg.get("gdn_noscan"):
                                l_[li]()
                        yield
                    prog["scan_done"] = s_

            SCN = dbg.get("gdn_scan_rate", 1)
            jstreams = [job_stream(0, 0), job_stream(1, STAG)]
            sstream = scan_stream()
            alive = [True, True]
            salive = [True]

            def step_scan():
                for _ in range(SCN):
                    if salive[0]:
                        try:
                            next(sstream)
                        except StopIteration:
                            salive[0] = False

            while any(alive) or salive[0]:
                for k_, g_ in enumerate(jstreams):
                    for rep in range(1):
                        if alive[k_]:
                            try:
                                next(g_)
                            except StopIteration:
                                alive[k_] = False
                        step_scan()
            if "gdnO" in dbg:
                o_ = P.dram("D_o%d" % h, [128, 32, 128], F32)
                P.dma("sp", o_, o_acc[:])
            def mk_epi(h, q4):
                def run():
                    n0 = q4 * 8
                    gt_, sq_ = gate_s[q4 % 2], ssq_s[q4 % 2]
                    oa = o_acc[:, n0:n0 + 8, :]
                    P.dma("sp", gt_[:], S_gate[n0 * 128:(n0 + 8) * 128, h * 128:(h + 1) * 128].rearrange("(n p) c -> p n c", p=128))
                    P.tt("dve", sq_[:], oa, oa, ALU.mult)
                    P.reduce("dve", rsn[:, n0:n0 + 8], sq_[:], ALU.add)
                    P.rsqrt(rsn[:, n0:n0 + 8], rsn[:, n0:n0 + 8], scale=1.0 / 128, bias=EPS)
                    P.tt("dve", sq_[:], oa, ap3(rsn[:, n0:n0 + 8], last=128), ALU.mult)
                    P.tt("pool", sq_[:], sq_[:], ap3(gnb[:, :], mid=8), ALU.mult)
                    P.act(gt_[:], gt_[:], AF.Silu)
                    P.tt("dve", ybfs[q4 % 2][:], sq_[:], gt_[:], ALU.mult)
                    P.dma("sp", S_ymix[n0 * 128:(n0 + 8) * 128, h * 128:(h + 1) * 128].rearrange("(n p) c -> p n c", p=128),
                          ybfs[q4 % 2][:])
                return run
            for q4 in range(4):
                pending_epi.append(mk_epi(h, q4))
            if h == list(heads)[-1]:
                while pending_epi:
                    pending_epi.pop(0)()
    P.barrier()


def na_tiles(a):
    if a == 0:
        return [(0 + i, i) for i in range(4)]
    if a == 1:
        return [(4 + i, i) for i in range(4)]
    if a == 30:
        return [(13 + i, 28 + i) for i in range(4)]
    if a == 31:
        return [(17 + i, 28 + i) for i in range(4)]
    return [(8 + i, a - 2 + i) for i in range(5)]


def build_na(P, nc, I, dbg, S_naqT, S_nakT, S_nav, S_ymix):
    inp = lambda n, s_, dt=F32: I.setdefault(n, nc.dram_tensor(n, list(s_), dt, kind="ExternalInput").ap())
    nab_d = inp("na_bias", [8, 128, 21, 128])
    nam_d = inp("na_mask", [128, 21, 128])
    NP = TT // 128
    with ExitStack() as st:
        mask = P.sb(st, [128, 21, 128], F32, "namask")
        P.dma("sp", mask[:], nam_d[:, :, :])
        BTs = [P.sb(st, [128, 21, 128], F32, "BT") for _ in range(2)]
        QTs = [P.sb(st, [64, T], BF16, "QT") for _ in range(2)]
        KTs = [P.sb(st, [64, TT], BF16, "KT") for _ in range(2)]
        V1s = [P.sb(st, [128, NP, 65], BF16, "V1") for _ in range(2)]
        for v_ in V1s:
            P.memset("pool", v_[:, :, 64:65], 1.0)
        yna = P.sb(st, [128, 32, 512], BF16, "yna")
        sbs = [P.sb(st, [128, 7, 128], F32, "nsb") for _ in range(2)]
        PTs = [P.sb(st, [128, 7, 128], BF16, "nPT") for _ in range(2)]
        rec = [P.sb(st, [128, 1], F32, "nrec") for _ in range(2)]
        stA = [P.ps(st, [128, 4, 128], F32, "stA") for _ in range(2)]
        stB = [P.ps(st, [128, 4, 128], F32, "stB") for _ in range(2)]
        pvo = [P.ps(st, [128, 512], F32, "pvo") for _ in range(2)]
        def head_loads(h):
            BT, QT, KT, V1 = BTs[h % 2], QTs[h % 2], KTs[h % 2], V1s[h % 2]
            P.dma("sp", BT[:], nab_d[h])
            P.dma("sp", QT[:], S_naqT[h])
            P.dma("sp", KT[:], S_nakT[h])
            P.dma("act", V1[:, :, 0:64], S_nav[:, h * 64:(h + 1) * 64].rearrange("(n p) c -> p n c", p=128))
            P.tt("pool", BT[:], BT[:], mask[:], ALU.add)

        def blkinfo(i):
            h, a = divmod(i, 32)
            tl = na_tiles(a)
            chunks = [m for (_, m) in tl] + [32, 33]
            return h, a, tl, len(tl), chunks

        def q0(i):
            h, a, tl, L, chunks = blkinfo(i)
            if i == 0:
                head_loads(0)
            if a == 8 and h + 1 < 8:
                head_loads(h + 1)
            QT, KT = QTs[h % 2], KTs[h % 2]
            sA, sB = stA[i % 2], stB[i % 2]
            q_ap = QT[:, a * 128:(a + 1) * 128]
            for j, m in enumerate(chunks):
                k0 = m * 128 if m < 32 else T + (m - 32) * 128
                dst = sA[:, j, :] if j < 4 else sB[:, j - 4, :]
                P.mm(dst, KT[:, k0:k0 + 128], q_ap)

        def q1(i):
            h, a, tl, L, chunks = blkinfo(i)
            BT = BTs[h % 2]
            sA, sB, sb_ = stA[i % 2], stB[i % 2], sbs[i % 2]
            t0 = tl[0][0]
            P.stt("dve", sb_[:, 0:4, :], sA[:, 0:4, :], 60.0, BT[:, t0:t0 + 4, :], ALU.min, ALU.add)
            if L == 5:
                P.stt("dve", sb_[:, 4:5, :], sB[:, 0:1, :], 60.0, BT[:, t0 + 4:t0 + 5, :], ALU.min, ALU.add)
                P.ts("dve", sb_[:, 5:7, :], sB[:, 1:3, :], 60.0, None, ALU.min)
            else:
                P.ts("dve", sb_[:, 4:6, :], sB[:, 0:2, :], 60.0, None, ALU.min)

        def q2(i):
            h, a, tl, L, chunks = blkinfo(i)
            P.act(PTs[i % 2][:, 0:L + 2, :], sbs[i % 2][:, 0:L + 2, :], AF.Exp)

        def q3(i):
            h, a, tl, L, chunks = blkinfo(i)
            V1, PT, po = V1s[h % 2], PTs[i % 2], pvo[i % 2]
            for j, m in enumerate(chunks):
                P.mm(po[:, 0:65], PT[:, j, :], V1[:, m, :], start=(j == 0), stop=(j == L + 1))

        def q4(i):
            h, a, tl, L, chunks = blkinfo(i)
            po = pvo[i % 2]
            P.recip(rec[i % 2][:], po[:, 64:65])
            P.act(yna[:, a, h * 64:(h + 1) * 64], po[:, 0:64], AF.Copy, scale=rec[i % 2][:, 0:1])

        skew([q0, q1, q2, q3, q4], 8 * 32)
        P.dma("sp", S_ymix[:, 512:1024].rearrange("(n p) c -> p n c", p=128), yna[:])
    P.barrier()


def build_tail(P, nc, I, dbg, x, out, S_mod, S_ymix, identf, identb):
    inp = lambda n, s_, dt=F32: I.setdefault(n, nc.dram_tensor(n, list(s_), dt, kind="ExternalInput").ap())
    w_out = inp("w_out", [D, D])
    wr_d = inp("w_router_l", [128, 8, NE])
    norm_ffn = I["norm_ffn"]
    fnorm_d = inp("final_norm", [1, D])
    w_gate = inp("w_gate", [NE, D, D])
    w_up = inp("w_up", [NE, D, D])
    w_down = inp("w_down", [NE, D, D])
    S_x1 = P.dram("S_x1", [T, D], F32)
    S_h2 = P.dram("S_h2", [T, D], BF16)
    keep = ExitStack()
    affT = P.sb(keep, [NE, T], F32, "affT")
    gt2 = P.sb(keep, [128, D], F32, "gt2")
    idxT = P.sb(keep, [128, 4, NE], U32, "idxT")
    valT = P.sb(keep, [128, 4, NE], F32, "valT")
    P.dma("sp", gt2[:], bcast_rows(S_mod[0:1, 5 * D:6 * D], 128))
    with ExitStack() as st:
        def bc_tile(row_ap, name):
            t_ = P.sb(st, [128, D], F32, name)
            P.dma("sp", t_[:], bcast_rows(row_ap, 128))
            return t_
        gt1 = bc_tile(S_mod[0:1, 2 * D:3 * D], "gt1")
        sh2 = bc_tile(S_mod[0:1, 3 * D:4 * D], "sh2")
        gm2 = bc_tile(S_mod[0:1, 4 * D:5 * D], "gm2")
        nf = bc_tile(norm_ffn[0:1, :], "nf")
        P.stt("dve", gm2[:], gm2[:], 1.0, nf[:], ALU.add, ALU.mult)
        wo = P.sb(st, [128, 8, D], BF16, "wo")
        for kc in range(8):
            P.dma("pool", wo[:, kc, :], w_out[kc * 128:(kc + 1) * 128, :])
        wr = P.sb(st, [128, 8, NE], F32, "wr")
        P.dma("sp", wr[:], wr_d[:, :, :])
        RG = 4
        yms = [P.sb(st, [128, D], BF16, "ym") for _ in range(RG)]
        ymT = [P.sb(st, [128, 8, 128], BF16, "ymT") for _ in range(RG)]
        xts = [P.sb(st, [128, D], F32, "xt4") for _ in range(RG)]
        x1s = [P.sb(st, [128, D], F32, "x1") for _ in range(RG)]
        tmps = [P.sb(st, [128, D], F32, "tmp4") for _ in range(RG)]
        h2s = [P.sb(st, [128, D], F32, "h2") for _ in range(RG)]
        h2b = [P.sb(st, [128, D], BF16, "h2b") for _ in range(RG)]
        h2T = [P.sb(st, [128, 8, 128], F32, "h2T") for _ in range(2)]
        junk = P.sb(st, [128, D], BF16, "junk4")
        sml = [P.sb(st, [128, 8], F32, "sml") for _ in range(RG)]
        lg = [P.sb(st, [128, NE], F32, "lg") for _ in range(RG)]
        trp = P.ps(st, [128, 8, 128], BF16, "trp4")
        yps = [P.ps(st, [128, 512], F32, "yps") for _ in range(2)]
        tps = [P.ps(st, [128, 4, 128], F32, "tps") for _ in range(2)]
        lps = [P.ps(st, [128, 512], F32, "lps") for _ in range(2)]
        aps_ = P.ps(st, [128, 512], F32, "aps")

        def pA0(i):
            r_ = i % RG
            P.dma("sp", yms[r_][:], S_ymix[i * 128:(i + 1) * 128, :])
            P.dma("sp", xts[r_][:], x[i * 128:(i + 1) * 128, :])

        def pA1(i):
            r_ = i % RG
            ym, yT = yms[r_], ymT[r_]
            for kc in range(8):
                P.tr(trp[:, kc, :], ym[:, kc * 128:(kc + 1) * 128], identb[:])
            P.copy("act", yT[:], trp[:])

        def pA2(i):
            r_ = i % RG
            yT, xt, x1, tmp = ymT[r_], xts[r_], x1s[r_], tmps[r_]
            for hf in range(2):
                for kc in range(8):
                    P.mm(yps[hf][:], yT[:, kc, :], wo[:, kc, hf * 512:(hf + 1) * 512], start=(kc == 0), stop=(kc == 7))
                P.tt("dve", tmp[:, hf * 512:(hf + 1) * 512], yps[hf][:], gt1[:, hf * 512:(hf + 1) * 512], ALU.mult)
            P.tt("pool", x1[:], tmp[:], xt[:], ALU.add)

        def pA3(i):
            r_ = i % RG
            x1, sm = x1s[r_], sml[r_]
            P.dma("sp", S_x1[i * 128:(i + 1) * 128, :], x1[:])
            P.act(junk[:], x1[:], AF.Square, accum_out=sm[:, 0:1])
            P.act(sm[:, 1:2], sm[:, 0:1], AF.Sqrt, bias=EPS, scale=1.0 / D)

        def pA4(i):
            r_ = i % RG
            x1, sm, tmp, h2, hb = x1s[r_], sml[r_], tmps[r_], h2s[r_], h2b[r_]
            P.recip(sm[:, 1:2], sm[:, 1:2])
            P.stt("dve", tmp[:], x1[:], sm[:, 1:2], gm2[:], ALU.mult, ALU.mult)
            P.tt("pool", h2[:], tmp[:], sh2[:], ALU.add)
            P.copy("pool", hb[:], h2[:])

        def pB1(i):
            r_ = i % RG
            h2, hT = h2s[r_], h2T[i % 2]
            P.dma("sp", S_h2[i * 128:(i + 1) * 128, :], h2b[r_][:])
            for kc in range(8):
                P.tr(tps[kc // 4][:, kc % 4, :], h2[:, kc * 128:(kc + 1) * 128], identf[:])
            P.copy("act", hT[:, 0:4, :], tps[0][:])
            P.copy("dve", hT[:, 4:8, :], tps[1][:])

        def pB2(i):
            r_ = i % RG
            hT, sm, lp = h2T[i % 2], sml[r_], lps[i % 2]
            for kc in range(8):
                P.mm(lp[:, 0:NE], hT[:, kc, :], wr[:, kc, :], start=(kc == 0), stop=(kc == 7))
            P.reduce("dve", sm[:, 2:3], lp[:, 0:NE], ALU.max)
            P.ts("dve", sm[:, 3:4], sm[:, 2:3], -1.0, None, ALU.mult)

        def pB3(i):
            r_ = i % RG
            sm, lp = sml[r_], lps[i % 2]
            P.act(lg[r_][:], lp[:, 0:NE], AF.Exp, bias=sm[:, 3:4], scale=1.0, accum_out=sm[:, 4:5])

        def pB4(i):
            r_ = i % RG
            sm = sml[r_]
            P.recip(sm[:, 5:6], sm[:, 4:5])
            P.ts("dve", lg[r_][:], lg[r_][:], sm[:, 5:6], None, ALU.mult)
            P.tr(aps_[0:NE, 0:128], lg[r_][:], identf[:])
            P.copy("act", affT[:, i * 128:(i + 1) * 128], aps_[0:NE, 0:128])

        skew([pA0, pA1, pA2, pA3, pA4, pB1, pB2, pB3, pB4], T // 128)
    P.barrier()
    if "tail4" in dbg:
        o_ = P.dram("D_affT", [NE, T], F32)
        P.dma("sp", o_, affT[:])
        return
    wst = ExitStack()
    wg = [P.sb(wst, [128, 8, D], BF16, "wg") for _ in range(2)]
    wu = [P.sb(wst, [128, 8, D], BF16, "wu") for _ in range(2)]
    wd = [P.sb(wst, [128, 8, D], BF16, "wd") for _ in range(2)]
    nexp = dbg.get("nexp", NE)

    def load_w(e, which=("g", "u", "d")):
        s_ = e % 2
        for (nm, dst, src) in (("g", wg[s_], w_gate), ("u", wu[s_], w_up), ("d", wd[s_], w_down)):
            if nm not in which:
                continue
            for hf in range(2):
                P.dma("pool", dst[:, hf * 4:(hf + 1) * 4, :],
                      src[e, hf * 512:(hf + 1) * 512, :].rearrange("(kc p) f -> p kc f", p=128))

    if "tail4" not in dbg and "topk" not in dbg:
        load_w(0)
        if nexp > 1:
            load_w(1)
    with ExitStack() as st:
        vals = P.sb(st, [NE, CAP], F32, "vals")
        idxs = P.sb(st, [NE, CAP], U32, "idxs")
        idxf = P.sb(st, [NE, CAP], F32, "idxf")
        S_aff = P.dram("S_aff", [NE, T], F32)
        S_cand = P.dram("S_cand", [128, 128], F32)
        P.dma("sp", S_aff[:, :], affT[:])
        a1 = P.sb(st, [128, 512], F32, "a1")
        c1 = P.sb(st, [128, 128], F32, "c1")
        c2 = P.sb(st, [NE, 1024], F32, "c2")
        P.dma("sp", a1[:], S_aff.rearrange("e (s t) -> (e s) t", t=512))
        for r_ in range(16):
            P.max8(c1[:, r_ * 8:(r_ + 1) * 8], a1[:])
            if r_ < 15:
                P.match_replace(a1[:], c1[:, r_ * 8:(r_ + 1) * 8], a1[:], -1.0)
        P.dma("sp", S_cand[:, :], c1[:])
        P.dma("sp", c2[:], S_cand.rearrange("(e s) r -> e (s r)", s=8))
        for r_ in range(CAP // 8):
            P.max8(vals[:, r_ * 8:(r_ + 1) * 8], c2[:])
            if r_ < CAP // 8 - 1:
                P.match_replace(c2[:], vals[:, r_ * 8:(r_ + 1) * 8], c2[:], -1.0)
        S_vals = P.dram("S_vals", [NE, CAP], F32)
        S_idx = P.dram("S_idx", [128, 64], U32)
        affrep = P.sb(st, [128, T], F32, "affrep")
        vals2 = P.sb(st, [128, 64], F32, "vals2")
        idx2 = P.sb(st, [128, 64], U32, "idx2")
        for r_ in range(8):
            P.dma("sp" if r_ % 2 else "act", affrep[r_::8, :], S_aff[:, :])
        P.dma("sp", S_vals[:, :], vals[:])
        P.dma("sp", vals2[:], S_vals.rearrange("e (r c) -> (e r) c", c=64))
        for g_ in range(8):
            P.max_index(idx2[:, g_ * 8:(g_ + 1) * 8], vals2[:, g_ * 8:(g_ + 1) * 8], affrep[:])
        P.dma("sp", S_idx[:, :], idx2[:])
        P.dma("sp", idxs[:], S_idx.rearrange("(e r) c -> e (r c)", r=8))
        P.copy("dve", idxf[:], idxs[:])
        tp = P.ps(st, [128, 512], F32, "tpk")
        for j in range(4):
            P.tr(tp[:, j * NE:(j + 1) * NE], idxf[:, j * 128:(j + 1) * 128], identf[0:NE, 0:NE])
        P.copy("dve", idxT[:], tp[:, 0:4 * NE].rearrange("p (j e) -> p j e", e=NE))
        for j in range(4):
            P.tr(tp[:, j * NE:(j + 1) * NE], vals[:, j * 128:(j + 1) * 128], identf[0:NE, 0:NE])
        P.copy("dve", valT[:], tp[:, 0:4 * NE].rearrange("p (j e) -> p j e", e=NE))
    P.barrier()
    if "topk" in dbg:
        o_ = P.dram("D_idxT", [128, 4, NE], U32)
        P.dma("sp", o_, idxT[:])
        o_ = P.dram("D_valT", [128, 4, NE], F32)
        P.dma("sp", o_, valT[:])
        return
    with ExitStack() as st:
        xg = [[P.sb(st, [128, D], BF16, "xg") for _ in range(4)] for _ in range(2)]
        xT = [P.sb(st, [128, 8, CAP], BF16, "xT") for _ in range(2)]
        hid = [P.sb(st, [128, 8, CAP], BF16, "hid") for _ in range(2)]
        sg = [P.sb(st, [128, CAP], F32, "sg") for _ in range(2)]
        ye = [P.sb(st, [128, D], F32, "ye") for _ in range(3)]
        trp = [P.ps(st, [128, 8, 128], BF16, "trpm") for _ in range(2)]
        pg = [P.ps(st, [128, 512], F32, "pg") for _ in range(2)]
        pu = [P.ps(st, [128, 512], F32, "pu") for _ in range(2)]
        py = [P.ps(st, [128, 512], F32, "py") for _ in range(2)]
        cnt = dict(y=0, p=0)
        sc_prev = []
        sc_cur = []

        def ph_g(e):
            for j in range(4):
                P.gather(xg[e % 2][j][:], S_h2[:, :], idxT[:, j, e:e + 1])

        def ph_t(e):
            for j in range(4):
                tpp = trp[j % 2]
                for kc in range(8):
                    P.tr(tpp[:, kc, :], xg[e % 2][j][:, kc * 128:(kc + 1) * 128], identb[:])
                if j % 2:
                    P.copy("act", xT[e % 2][:, :, j * 128:(j + 1) * 128], tpp[:])
                else:
                    P.copy("dve", xT[e % 2][:, :, j * 128:(j + 1) * 128], tpp[:])

        def ph_u(e):
            s_ = e % 2
            for fc in range(8):
                r_ = fc % 2
                for kc in range(8):
                    P.mm(pg[r_][:], wg[s_][:, kc, fc * 128:(fc + 1) * 128], xT[s_][:, kc, :], start=(kc == 0), stop=(kc == 7))
                for kc in range(8):
                    P.mm(pu[r_][:], wu[s_][:, kc, fc * 128:(fc + 1) * 128], xT[s_][:, kc, :], start=(kc == 0), stop=(kc == 7))
                P.act(sg[r_][:], pg[r_][:], AF.Silu)
                P.tt("dve", hid[s_][:, fc, :], sg[r_][:], pu[r_][:], ALU.mult)

        def ph_d(e):
            s_ = e % 2
            sc_prev[:] = sc_cur
            sc_cur[:] = []
            for j in range(4):
                y_ = ye[cnt["y"] % 3]
                cnt["y"] += 1
                for hf in range(2):
                    cnt["p"] += 1
                    pp = py[cnt["p"] % 2]
                    for fc in range(8):
                        P.mm(pp[:], hid[s_][:, fc, j * 128:(j + 1) * 128], wd[s_][:, fc, hf * 512:(hf + 1) * 512],
                             start=(fc == 0), stop=(fc == 7))
                    P.stt("dve", y_[:, hf * 512:(hf + 1) * 512], pp[:], valT[:, j, e:e + 1], gt2[:, hf * 512:(hf + 1) * 512],
                          ALU.mult, ALU.mult)
                sc_cur.append(P.scatter_add(S_x1[:, :], y_[:], idxT[:, j, e:e + 1], after=list(sc_prev)))

        ph_g(0)
        ph_t(0)
        for e in range(nexp):
            if e + 1 < nexp:
                ph_g(e + 1)
            ph_u(e)
            if e + 2 < nexp:
                load_w(e + 2, ("g", "u"))
            if e + 1 < nexp:
                ph_t(e + 1)
            ph_d(e)
            if e + 2 < nexp:
                load_w(e + 2, ("d",))
    P.barrier()
    wst.close()
    if "moe" in dbg:
        return
    with ExitStack() as st:
        fn = P.sb(st, [128, D], F32, "fn")
        P.dma("sp", fn[:], bcast_rows(fnorm_d[0:1, :], 128))
        xs = [P.sb(st, [128, D], F32, "xf") for _ in range(4)]
        os_ = [P.sb(st, [128, D], F32, "of") for _ in range(3)]
        junk = P.sb(st, [128, D], BF16, "junkf")
        sm = [P.sb(st, [128, 2], F32, "smf") for _ in range(4)]

        def z0(i):
            P.dma("sp", xs[i % 4][:], S_x1[i * 128:(i + 1) * 128, :])

        def z1(i):
            s_ = sm[i % 4]
            P.act(junk[:], xs[i % 4][:], AF.Square, accum_out=s_[:, 0:1])
            P.act(s_[:, 1:2], s_[:, 0:1], AF.Sqrt, bias=EPS, scale=1.0 / D)

        def z2(i):
            s_ = sm[i % 4]
            P.recip(s_[:, 1:2], s_[:, 1:2])
            P.stt("dve", os_[i % 3][:], xs[i % 4][:], s_[:, 1:2], fn[:], ALU.mult, ALU.mult)

        def z3(i):
            P.dma("act", out[i * 128:(i + 1) * 128, :], os_[i % 3][:])

        skew([z0, z1, z2, z3], T // 128)
    keep.close()


def host_inputs(inputs, b):
    f = lambda a: np.ascontiguousarray(np.asarray(a), dtype=np.float32)
    m = {}
    m["x"] = f(inputs["x"][b])
    m["ctx"] = f(inputs["ctx"][b])
    cc = np.stack([np.asarray(inputs["c"][b]).reshape(8, 128).T, np.asarray(inputs["c_ctx"]).reshape(8, 128).T], axis=-1)
    m["cc"] = f(cc)
    m["w_mod"] = f(inputs["w_mod"][0])
    m["b_mod"] = f(inputs["b_mod"][0]).reshape(1, -1)
    m["norm_mix"] = f(inputs["norm_mix"][0]).reshape(1, -1)
    m["norm_ffn"] = f(inputs["norm_ffn"][0]).reshape(1, -1)
    m["w_in"] = f(inputs["w_in"][0])
    m["ident_f"] = np.eye(128, dtype=np.float32)
    na_index_tables()
    m.update({k_: v_ for k_, v_ in host_consts().items() if k_ != "na_idx"})
    m["cwl"] = f(np.asarray(inputs["conv_qkv"][0]).reshape(5, 12, 128).transpose(2, 1, 0))
    m["a_log"] = f(inputs["a_log"][0]).reshape(1, 8)
    m["dt_bias"] = f(inputs["dt_bias"][0]).reshape(1, 8)
    m["gdn_norm"] = f(inputs["gdn_norm"][0]).reshape(1, 128)
    m["w_out"] = f(inputs["w_out"][0])
    m["w_router_l"] = f(np.asarray(inputs["w_router"][0]).reshape(8, 128, NE).transpose(1, 0, 2))
    m["final_norm"] = f(inputs["final_norm"]).reshape(1, D)
    m["w_gate"] = f(inputs["w_gate"][0])
    m["w_up"] = f(inputs["w_up"][0])
    m["w_down"] = f(inputs["w_down"][0])
    ridx, cidx = na_index_tables()
    rpb = f(inputs["na_rpb"][0])
    m["na_bias"] = np.ascontiguousarray(rpb[:, ridx, cidx])
    return m


def na_index_tables():
    if "na_idx" in _CONSTS:
        return _CONSTS["na_idx"]
    ridx = np.zeros((128, 21, 128), np.int64)
    cidx = np.zeros((128, 21, 128), np.int64)
    mask = np.full((128, 21, 128), NEG, np.float32)
    key = np.arange(128)
    kr2, kc = key // 64, key % 64
    qr2, qc = key // 64, key % 64
    cs = np.clip(qc - 8, 0, 48)
    for a in (0, 1, 2, 30, 31):
        for (ti, m_) in na_tiles(a):
            krow = (2 * m_ + kr2)[:, None]
            qrow = (2 * a + qr2)[None, :]
            rs = np.clip(qrow - 4, 0, 56)
            vr = (krow >= rs) & (krow < rs + 8)
            vc = (kc[:, None] >= cs[None, :]) & (kc[:, None] < cs[None, :] + 16)
            valid = vr & vc
            ridx[:, ti, :] = np.clip(krow - qrow + 7, 0, 14)
            cidx[:, ti, :] = np.clip(kc[:, None] - qc[None, :] + 15, 0, 30)
            mask[:, ti, :] = np.where(valid, 0.0, NEG)
    _CONSTS["na_idx"] = (ridx, cidx)
    _CONSTS["na_mask"] = mask
    return _CONSTS["na_idx"]


_CONSTS = {}


def host_consts():
    if "cmat" in _CONSTS:
        return _CONSTS
    r = np.arange(128)[:, None]
    c = np.arange(128)[None, :]
    same = (r // 64) == (c // 64)
    cm = np.zeros((128, 8, 128), np.float32)
    cm[:, 0] = same & (r > c)
    cm[:, 1] = same & (r <= c)
    cm[:, 2] = same & (r < c)
    cm[:, 3] = same & (r >= c)
    cm[:, 4] = 1.0
    partner = np.where((np.arange(128) % 64) < 32, np.arange(128) + 32, np.arange(128) - 32)
    cm[partner, 5, np.arange(128)] = 1.0
    cm[:, 6] = (r < 64)
    cm[:, 7] = (r >= 64)
    _CONSTS["cmat"] = cm
    pairs = 32
    inv_freq = (np.float32(10000.0) ** (-np.arange(pairs, dtype=np.float32) / np.float32(pairs))).astype(np.float32)
    t = np.arange(T)
    pos_r = (t // 64).astype(np.float32)
    pos_c = (t % 64).astype(np.float32)
    cos = np.zeros((128, T), np.float32)
    sin = np.zeros((128, T), np.float32)
    for p in range(128):
        j = p % 32
        pos = pos_r if p < 64 else pos_c
        ang = (pos * inv_freq[j]).astype(np.float32)
        cos[p] = np.cos(ang).astype(np.float32)
        sgn = -1.0 if (p % 64) < 32 else 1.0
        sin[p] = sgn * np.sin(ang).astype(np.float32)
    _CONSTS["rope_cos"] = cos
    _CONSTS["rope_sin"] = sin
    return _CONSTS


_NC_CACHE = {}


def kernel(**inputs):
    if "nc" not in _NC_CACHE:
        nc = bass.Bass("TRN2", target_bir_lowering=False)
        P, I = build(nc)
        P.finish()
        _NC_CACHE["nc"] = (nc, sorted(I.keys()))
    nc, names = _NC_CACHE["nc"]
    in_maps = []
    for b in range(NCORES):
        full = host_inputs(inputs, b)
        in_maps.append({k: full[k] for k in names})
    res = run_bass_kernel_spmd(nc, in_maps, core_ids=list(range(NCORES)))
    outs = [np.asarray(res.results[b]["out"], dtype=np.float32) for b in range(NCORES)]
    return np.stack(outs, axis=0)
```
